# Optimizing a Trainium2 kernel written in Bass

```python
import jax
import jax.numpy as jnp
from jax import lax
import numpy as np


D_MODEL = 2048
BATCH = 4
SEQ = 4096
DEPTH = 2

N_EVEN = (DEPTH + 1) // 2
N_ODD = DEPTH // 2
ALPHA = (2 * DEPTH) ** 0.25
BETA = (8 * DEPTH) ** -0.25
GRID_W = 64
PLE_DIM = 256
LN_EPS = 1e-5
RMS_EPS = 1e-6

MLSTM_HEADS = 4
MLSTM_DV = D_MODEL // 2 // MLSTM_HEADS
MLSTM_DK = MLSTM_DV // 2
MLSTM_CHUNK = 64
GATE_CAP = 15.0

HGRN_DK = 128
HGRN_HEADS = D_MODEL // 2 // HGRN_DK
HGRN_DV = HGRN_DK
HGRN_CHUNK = 16

A_WIDTH = MLSTM_HEADS * MLSTM_DV
B_WIDTH = HGRN_HEADS * HGRN_DV
REC_SPLITS = (MLSTM_HEADS * MLSTM_DK, MLSTM_HEADS * MLSTM_DK, A_WIDTH, A_WIDTH, 4 * MLSTM_HEADS, HGRN_HEADS * HGRN_DK, HGRN_HEADS * HGRN_DK, HGRN_HEADS * HGRN_DK, B_WIDTH, B_WIDTH)
REC_IN = sum(REC_SPLITS)

NA_DH = 128
NA_HEADS = D_MODEL // NA_DH
NA_WIDTH = NA_HEADS * NA_DH
NA_KH = 8
NA_KW = 16

FFN_DIM = ((8 * D_MODEL // 3 + 255) // 256) * 256
N_EXPERTS = 8
TOP_K = 2
MOE_DIM = 7 * D_MODEL // 2
MOE_BLOCK = 512

kernel_name = 'hybrid_mlstm_hgrn2_natten_moe_encoder'


def layer_norm(x, w, b):
    xf = x.astype(jnp.float32)
    mu = jnp.mean(xf, axis=-1, keepdims=True)
    xc = xf - mu
    var = jnp.mean(xc * xc, axis=-1, keepdims=True)
    return (xc * lax.rsqrt(var + LN_EPS) * w.astype(jnp.float32) + b.astype(jnp.float32)).astype(x.dtype)


def head_rms_norm(h, w):
    wh = w.astype(jnp.float32).reshape(h.shape[1], 1, h.shape[-1])
    return h * lax.rsqrt(jnp.mean(h * h, axis=-1, keepdims=True) + RMS_EPS) * wh


def to_heads(z, nh):
    bsz, t, _ = z.shape
    return z.reshape(bsz, t, nh, -1).transpose(0, 2, 1, 3).astype(jnp.float32)


def from_heads(h):
    bsz, nh, t, d = h.shape
    return h.transpose(0, 2, 1, 3).reshape(bsz, t, nh * d)


def flip_t(z):
    return jnp.flip(z, axis=2)


def mlstm_chunkwise(q, k, v, log_i, log_f):
    bsz, nh, t, dk = q.shape
    dv = v.shape[-1]
    L = MLSTM_CHUNK
    nc = t // L
    q = q.reshape(bsz, nh, nc, L, dk)
    k = k.reshape(bsz, nh, nc, L, dk)
    v = v.reshape(bsz, nh, nc, L, dv)
    log_i = log_i.reshape(bsz, nh, nc, L)
    b = jnp.cumsum(log_f.reshape(bsz, nh, nc, L), axis=-1)
    g = b[..., -1]
    a = g[..., None] - b + log_i
    m_loc = jnp.max(a, axis=-1)
    kw = k * jnp.exp(a - m_loc[..., None])[..., None]
    c_loc = jnp.einsum('bhcsd,bhcse->bhcde', kw, v)
    n_loc = jnp.sum(kw, axis=-2)

    def step(carry, inp):
        c, n, m = carry
        c_l, n_l, m_l, g_c, q_c = inp
        y = jnp.einsum('bhtd,bhde->bhte', q_c, c)
        d = jnp.einsum('bhtd,bhd->bht', q_c, n)
        m_new = jnp.maximum(g_c + m, m_l)
        decay = jnp.exp(g_c + m - m_new)
        inj = jnp.exp(m_l - m_new)
        c = decay[..., None, None] * c + inj[..., None, None] * c_l
        n = decay[..., None] * n + inj[..., None] * n_l
        return (c, n, m_new), (y, d, m)

    init = (jnp.zeros((bsz, nh, dk, dv), jnp.float32), jnp.zeros((bsz, nh, dk), jnp.float32), jnp.zeros((bsz, nh), jnp.float32))
    xs = (jnp.moveaxis(c_loc, 2, 0), jnp.moveaxis(n_loc, 2, 0), jnp.moveaxis(m_loc, 2, 0), jnp.moveaxis(g, 2, 0), jnp.moveaxis(q, 2, 0))
    _, (y_inter, d_inter, m_prev) = lax.scan(step, init, xs)
    y_inter = jnp.moveaxis(y_inter, 0, 2)
    d_inter = jnp.moveaxis(d_inter, 0, 2)
    m_prev = jnp.moveaxis(m_prev, 0, 2)

    causal = jnp.tril(jnp.ones((L, L), dtype=bool))
    log_d = jnp.where(causal, b[..., :, None] - b[..., None, :] + log_i[..., None, :], -jnp.inf)
    inter_log = b + m_prev[..., None]
    m_t = jnp.maximum(jnp.max(log_d, axis=-1), inter_log)
    s = jnp.einsum('bhctd,bhcsd->bhcts', q, k) * jnp.exp(log_d - m_t[..., None])
    inter_w = jnp.exp(inter_log - m_t)
    num = jnp.einsum('bhcts,bhcse->bhcte', s, v) + inter_w[..., None] * y_inter
    den = jnp.sum(s, axis=-1) + inter_w * d_inter
    h = num / jnp.maximum(jnp.abs(den), jnp.exp(-m_t))[..., None]
    return h.reshape(bsz, nh, t, dv)


def hgrn2_chunkwise(q, k, v, log_f):
    bsz, nh, t, dk = q.shape
    dv = v.shape[-1]
    L = HGRN_CHUNK
    nc = t // L
    q = q.reshape(bsz, nh, nc, L, dk)
    k = k.reshape(bsz, nh, nc, L, dk)
    v = v.reshape(bsz, nh, nc, L, dv)
    b = jnp.cumsum(log_f.reshape(bsz, nh, nc, L, dk), axis=-2)
    g = b[..., -1, :]
    q_in = q * jnp.exp(b)
    k_in = k * jnp.exp(-b)
    k_end = k * jnp.exp(g[..., None, :] - b)
    causal = jnp.tril(jnp.ones((L, L), dtype=bool))
    a = jnp.where(causal, jnp.einsum('bhctd,bhcsd->bhcts', q_in, k_in), 0.0)
    o_intra = jnp.einsum('bhcts,bhcse->bhcte', a, v)

    def step(s, inp):
        k_c, v_c, g_c, q_c = inp
        y = jnp.einsum('bhtd,bhde->bhte', q_c, s)
        s = jnp.exp(g_c)[..., None] * s + jnp.einsum('bhsd,bhse->bhde', k_c, v_c)
        return s, y

    xs = (jnp.moveaxis(k_end, 2, 0), jnp.moveaxis(v, 2, 0), jnp.moveaxis(g, 2, 0), jnp.moveaxis(q_in, 2, 0))
    _, y = lax.scan(step, jnp.zeros((bsz, nh, dk, dv), jnp.float32), xs)
    o = o_intra + jnp.moveaxis(y, 0, 2)
    return o.reshape(bsz, nh, t, dv)


def recurrent_mixer(x, w_in, gate_bias, mlstm_norm_w, lower_bound, hgrn_norm_w, w_out):
    bsz, t, _ = x.shape
    u = x @ w_in
    q_a, k_a, v_a, o_a, gates_a, q_b, f_fw, f_bw, i_b, g_b = jnp.split(u, [int(c) for c in np.cumsum(REC_SPLITS)[:-1]], axis=-1)

    qa = to_heads(q_a, MLSTM_HEADS) * (MLSTM_DK ** -0.5)
    ka = to_heads(k_a, MLSTM_HEADS)
    va = to_heads(v_a, MLSTM_HEADS)
    gates = gates_a.astype(jnp.float32).reshape(bsz, t, 4, MLSTM_HEADS) + gate_bias.astype(jnp.float32)
    gates = (GATE_CAP * jnp.tanh(gates / GATE_CAP)).transpose(2, 0, 3, 1)
    h_fw = mlstm_chunkwise(qa, ka, va, gates[0], jax.nn.log_sigmoid(gates[1]))
    h_bw = flip_t(mlstm_chunkwise(flip_t(qa), flip_t(ka), flip_t(va), flip_t(gates[2]), flip_t(jax.nn.log_sigmoid(gates[3]))))
    h_a = from_heads(head_rms_norm(h_fw + h_bw, mlstm_norm_w))
    y_a = h_a * jax.nn.sigmoid(o_a.astype(jnp.float32))

    qb = jax.nn.silu(to_heads(q_b, HGRN_HEADS))
    vb = to_heads(i_b, HGRN_HEADS)
    lb = lower_bound.astype(jnp.float32).reshape(2, HGRN_HEADS, 1, HGRN_DK)
    f_f = lb[0] + (1.0 - lb[0]) * jax.nn.sigmoid(to_heads(f_fw, HGRN_HEADS))
    f_b = lb[1] + (1.0 - lb[1]) * jax.nn.sigmoid(to_heads(f_bw, HGRN_HEADS))
    o_fw = hgrn2_chunkwise(qb, 1.0 - f_f, vb, jnp.log(f_f))
    o_bw = flip_t(hgrn2_chunkwise(flip_t(qb), flip_t(1.0 - f_b), flip_t(vb), flip_t(jnp.log(f_b))))
    h_b = from_heads(head_rms_norm(o_fw + o_bw, hgrn_norm_w))
    y_b = h_b * jax.nn.silu(g_b.astype(jnp.float32))

    y = jnp.concatenate([y_a, y_b], axis=-1).astype(x.dtype)
    return y @ w_out


def neighborhood_attention(x, w_qkv, rpb, w_out):
    bsz, t, _ = x.shape
    rows = t // GRID_W
    kh = min(NA_KH, rows)
    qkv = (x @ w_qkv).reshape(bsz, rows, GRID_W, 3, NA_HEADS, NA_DH)
    q = qkv[:, :, :, 0].transpose(1, 0, 3, 2, 4)
    k = qkv[:, :, :, 1].transpose(0, 3, 1, 2, 4)
    v = qkv[:, :, :, 2].transpose(0, 3, 1, 2, 4)
    row_start = jnp.clip(jnp.arange(rows) - kh // 2, 0, rows - kh)
    col = jnp.arange(GRID_W)
    col_start = jnp.clip(col - NA_KW // 2, 0, GRID_W - NA_KW)
    col_mask = (col[None, :] >= col_start[:, None]) & (col[None, :] < col_start[:, None] + NA_KW)
    dc_idx = jnp.clip(col[None, :] - col[:, None] + NA_KW - 1, 0, 2 * NA_KW - 2)
    rpb_cols = rpb[:, :, dc_idx]
    scale = NA_DH ** -0.5

    def attend_row(args):
        q_r, r, rs = args
        k_r = lax.dynamic_slice_in_dim(k, rs, kh, axis=2)
        v_r = lax.dynamic_slice_in_dim(v, rs, kh, axis=2)
        dr_idx = rs + jnp.arange(kh) - r + NA_KH - 1
        bias = rpb_cols[:, dr_idx].transpose(0, 2, 1, 3).astype(jnp.float32)
        s = jnp.einsum('bhqd,bhjkd->bhqjk', q_r, k_r).astype(jnp.float32) * scale + bias[None]
        s = jnp.where(col_mask[None, None, :, None, :], s, -jnp.inf)
        pr = jax.nn.softmax(s.reshape(bsz, NA_HEADS, GRID_W, kh * GRID_W), axis=-1).reshape(s.shape).astype(v.dtype)
        return jnp.einsum('bhqjk,bhjkd->bhqd', pr, v_r)

    out = lax.map(attend_row, (q, jnp.arange(rows), row_start))
    out = out.transpose(1, 0, 3, 2, 4).reshape(bsz, t, NA_WIDTH)
    return out @ w_out


def swiglu(x, w_gate, w_up, w_down):
    return (jax.nn.silu(x @ w_gate) * (x @ w_up)) @ w_down


def moe_swiglu(x, w_router, b_router, w_gate, w_up, w_down):
    bsz, t, d = x.shape
    xt = x.reshape(-1, d)
    n = xt.shape[0]
    nk = n * TOP_K
    logits = (xt @ w_router).astype(jnp.float32) + b_router.astype(jnp.float32)
    top_logit, top_e = lax.top_k(logits, TOP_K)
    gate = jax.nn.softmax(top_logit, axis=-1)
    e_flat = top_e.reshape(-1).astype(jnp.int32)
    slot = jnp.arange(nk, dtype=jnp.int32)
    order = jnp.argsort(e_flat * nk + slot)
    e_sorted = e_flat[order]
    tok_sorted = order // TOP_K
    g_sorted = gate.reshape(-1)[order]
    counts = jnp.bincount(e_flat, length=N_EXPERTS)
    padded = (counts + MOE_BLOCK - 1) // MOE_BLOCK * MOE_BLOCK
    pad_end = jnp.cumsum(padded)
    pad_start = pad_end - padded
    start = jnp.cumsum(counts) - counts
    dest = pad_start[e_sorted] + slot - start[e_sorted]
    n_blocks = -(-(nk + N_EXPERTS * (MOE_BLOCK - 1)) // MOE_BLOCK)
    xs = jnp.zeros((n_blocks * MOE_BLOCK, d), x.dtype).at[dest].set(xt[tok_sorted])
    block_e = jnp.minimum(jnp.searchsorted(pad_end, jnp.arange(n_blocks) * MOE_BLOCK, side='right'), N_EXPERTS - 1)

    def run_block(args):
        xb, e = args
        return swiglu(xb, w_gate[e], w_up[e], w_down[e])

    ys = lax.map(run_block, (xs.reshape(n_blocks, MOE_BLOCK, d), block_e)).reshape(-1, d)
    out = jnp.zeros_like(xt).at[tok_sorted].add(ys[dest] * g_sorted[:, None].astype(x.dtype))
    return out.reshape(bsz, t, d)


def setup_inputs(seed: int = 0) -> dict:
    key = jax.random.key(seed)
    ks = jax.random.split(key, 24)
    f32 = jnp.float32

    def nrm(k, shape, scale):
        return jax.random.normal(k, shape, f32) * scale

    gate_offset = jnp.stack([jnp.full((MLSTM_HEADS,), -1.0, f32), jnp.linspace(3.0, 6.0, MLSTM_HEADS, dtype=f32), jnp.full((MLSTM_HEADS,), -1.0, f32), jnp.linspace(3.0, 6.0, MLSTM_HEADS, dtype=f32)])
    return {
        'x': nrm(ks[0], (BATCH, SEQ, D_MODEL), 1.0),
        'p': nrm(ks[1], (DEPTH, BATCH, SEQ, PLE_DIM), 1.0),
        'ln_w': 1.0 + nrm(ks[2], (DEPTH, 2, D_MODEL), 0.02),
        'ln_b': nrm(ks[3], (DEPTH, 2, D_MODEL), 0.02),
        'rec_w_in': nrm(ks[4], (N_EVEN, D_MODEL, REC_IN), D_MODEL ** -0.5),
        'mlstm_gate_bias': gate_offset[None] + nrm(ks[5], (N_EVEN, 4, MLSTM_HEADS), 0.1),
        'mlstm_norm_w': 1.0 + nrm(ks[6], (N_EVEN, A_WIDTH), 0.02),
        'hgrn_lb_logits': nrm(ks[7], (2, N_EVEN + 1, HGRN_HEADS * HGRN_DK), 0.1),
        'hgrn_norm_w': 1.0 + nrm(ks[8], (N_EVEN, B_WIDTH), 0.02),
        'rec_w_out': nrm(ks[9], (N_EVEN, A_WIDTH + B_WIDTH, D_MODEL), BETA * (A_WIDTH + B_WIDTH) ** -0.5),
        'ffn_w_gate': nrm(ks[10], (N_EVEN, D_MODEL, FFN_DIM), D_MODEL ** -0.5),
        'ffn_w_up': nrm(ks[11], (N_EVEN, D_MODEL, FFN_DIM), D_MODEL ** -0.5),
        'ffn_w_down': nrm(ks[12], (N_EVEN, FFN_DIM, D_MODEL), BETA * FFN_DIM ** -0.5),
        'na_w_qkv': nrm(ks[13], (N_ODD, D_MODEL, 3 * NA_WIDTH), D_MODEL ** -0.5),
        'na_rpb': nrm(ks[14], (N_ODD, NA_HEADS, 2 * NA_KH - 1, 2 * NA_KW - 1), 0.1),
        'na_w_out': nrm(ks[15], (N_ODD, NA_WIDTH, D_MODEL), BETA * NA_WIDTH ** -0.5),
        'moe_w_router': nrm(ks[16], (N_ODD, D_MODEL, N_EXPERTS), D_MODEL ** -0.5),
        'moe_b_router': nrm(ks[17], (N_ODD, N_EXPERTS), 0.01),
        'moe_w_gate': nrm(ks[18], (N_ODD, N_EXPERTS, D_MODEL, MOE_DIM), D_MODEL ** -0.5),
        'moe_w_up': nrm(ks[19], (N_ODD, N_EXPERTS, D_MODEL, MOE_DIM), D_MODEL ** -0.5),
        'moe_w_down': nrm(ks[20], (N_ODD, N_EXPERTS, MOE_DIM, D_MODEL), BETA * MOE_DIM ** -0.5),
        'ple_w_gate': nrm(ks[21], (DEPTH, D_MODEL, D_MODEL), D_MODEL ** -0.5),
        'ple_w_proj': nrm(ks[22], (DEPTH, PLE_DIM, D_MODEL), PLE_DIM ** -0.5),
    }


def reference(x, p, ln_w, ln_b, rec_w_in, mlstm_gate_bias, mlstm_norm_w, hgrn_lb_logits, hgrn_norm_w, rec_w_out, ffn_w_gate, ffn_w_up, ffn_w_down, na_w_qkv, na_rpb, na_w_out, moe_w_router, moe_b_router, moe_w_gate, moe_w_up, moe_w_down, ple_w_gate, ple_w_proj):
    lower_bounds = jnp.cumsum(jax.nn.softmax(hgrn_lb_logits.astype(jnp.float32), axis=1), axis=1)
    for i in range(DEPTH):
        j = i // 2
        if i % 2 == 0:
            mix = recurrent_mixer(x, rec_w_in[j], mlstm_gate_bias[j], mlstm_norm_w[j], lower_bounds[:, j], hgrn_norm_w[j], rec_w_out[j])
        else:
            mix = neighborhood_attention(x, na_w_qkv[j], na_rpb[j], na_w_out[j])
        x = layer_norm(ALPHA * x + mix, ln_w[i, 0], ln_b[i, 0])
        if i % 2 == 0:
            ffn = swiglu(x, ffn_w_gate[j], ffn_w_up[j], ffn_w_down[j])
        else:
            ffn = moe_swiglu(x, moe_w_router[j], moe_b_router[j], moe_w_gate[j], moe_w_up[j], moe_w_down[j])
        x = layer_norm(ALPHA * x + ffn, ln_w[i, 1], ln_b[i, 1])
        x = x + jax.nn.sigmoid(x @ ple_w_gate[i]) * (p[i] @ ple_w_proj[i])
    return x
```

```python
import numpy as np
import concourse.bass as bass
import concourse.mybir as mybir
from concourse.bass_utils import run_bass_kernel_spmd
from contextlib import ExitStack

F32 = mybir.dt.float32
BF16 = mybir.dt.bfloat16
U32 = mybir.dt.uint32
AF = mybir.ActivationFunctionType
ALU = mybir.AluOpType
AX = mybir.AxisListType

D = 2048
T = 4096
F = 2304
O = 2048
KC = D // 128
CH = 64
NCF = F // CH
NCT = T // CH
REC_IN = 8208
FFN = 5632
MOE = 7168
NE = 8
SK_O, SK_E = 1, 2
SK_P = 2
NBK = 4
CAP = 640
ALPHA = 4 ** 0.25
LN_EPS = 1e-5
RMS_EPS = 1e-6


class Buf:
    __slots__ = ("name", "w", "readers", "dsem", "dcnt", "rel")

    def __init__(self, name):
        self.name = name
        self.w = {}
        self.readers = {}
        self.dsem = None
        self.dcnt = 0
        self.rel = False


class MK:
    def __init__(self, nc, es):
        self.nc = nc
        self.es = es
        self.eng = dict(pe=nc.tensor, act=nc.scalar, dve=nc.vector, pool=nc.gpsimd, sp=nc.sync)
        self.sem = {k: es.enter_context(nc.semaphore("sem_" + k)) for k in self.eng}
        self.cnt = {k: 0 for k in self.eng}
        self.seen = {k: {} for k in self.eng}
        self.owners = []
        self.free_sems = []
        self.nsem = 0
        self.nb = 0

    def buf(self, name=None):
        self.nb += 1
        return Buf(name or ("b%d" % self.nb))

    def _acquire(self, owner):
        if owner.dsem is None or owner.rel:
            if self.free_sems:
                owner.dsem, owner.dcnt = self.free_sems.pop()
            else:
                owner.dsem = self.es.enter_context(self.nc.semaphore("ds%d" % self.nsem))
                self.nsem += 1
                owner.dcnt = 0
            owner.rel = False
            self.owners.append(owner)
            return True
        return False

    def _deps(self, reads, writes):
        need = {}
        for b in reads:
            for k, v in b.w.items():
                if need.get(k, 0) < v:
                    need[k] = v
        for b in writes:
            for k, v in b.w.items():
                if need.get(k, 0) < v:
                    need[k] = v
            for k, v in b.readers.items():
                if need.get(k, 0) < v:
                    need[k] = v
        return need

    def _wait(self, e, need):
        for key, val in need.items():
            if key[0] == "e" and key[1] == "pe" and e == "pe":
                continue
            if self.seen[e].get(key, 0) >= val:
                continue
            sem = self.sem[key[1]] if key[0] == "e" else key[1].dsem
            self.eng[e].wait_ge(sem, val)
            self.seen[e][key] = val

    def _record(self, key, val, reads, writes):
        for b in reads:
            if b.readers.get(key, 0) < val:
                b.readers[key] = val
        for b in writes:
            b.w[key] = val
            b.readers = {}

    def op(self, e, fn, reads=(), writes=(), sig=True):
        self._wait(e, self._deps(reads, writes))
        ins = fn(self.eng[e])
        if sig:
            self.cnt[e] += 1
            ins.then_inc(self.sem[e], 1)
            t = self.cnt[e]
        else:
            t = self.cnt[e] + 1
        self._record(("e", e), t, reads, writes)
        return ins

    def dma(self, q, out, in_, reads, writes, owner, **kw):
        need = self._deps(reads, writes)
        fresh = self._acquire(owner)
        if owner.dcnt and not fresh:
            k = ("d", owner)
            if need.get(k, 0) < owner.dcnt * 16:
                need[k] = owner.dcnt * 16
        self._wait(q, need)
        ins = self.eng[q].dma_start(out=out, in_=in_, **kw)
        owner.dcnt += 1
        ins.then_inc(owner.dsem, 16)
        self._record(("d", owner), owner.dcnt * 16, reads, writes)
        return ins

    def barrier(self):
        need = {}
        for e in self.eng:
            if e != "sp" and self.cnt[e]:
                need[("e", e)] = self.cnt[e]
        for o in self.owners:
            if o.dcnt:
                need[("d", o)] = o.dcnt * 16
        self._wait("sp", need)
        self.nc.sync.sem_inc(self.sem["sp"], 1)
        self.cnt["sp"] += 1
        for e in self.eng:
            if e != "sp":
                self.eng[e].wait_ge(self.sem["sp"], self.cnt["sp"])
                self.seen[e][("e", "sp")] = self.cnt["sp"]
        for o in self.owners:
            o.rel = True
            self.free_sems.append((o.dsem, o.dcnt))
        self.owners = []

    def finish(self, bufs):
        need = {}
        for b in bufs:
            for k, v in b.w.items():
                need[k] = max(need.get(k, 0), v)
        self._wait("sp", need)


C_QA, C_KA, C_VA, C_OA, C_GI, C_GF, C_QB, C_FF, C_FB, C_IB, C_GB = (
    0, 512, 1024, 2048, 3072, 3080, 3088, 4112, 5136, 6160, 7184)


class Prog:
    def __init__(self, nc, es, dbg=(), stop=None):
        self.nc = nc
        self.es = es
        self.mk = MK(nc, es)
        self.dbg = set(dbg)
        self.stop = stop
        self.outs = []
        self.inp = {}
        self.scr = {}
        self.scrb = {}
        mk = self.mk
        self.ps = [es.enter_context(nc.psum_tensor("ps%d" % i, [128, 512], F32)) for i in range(4)]
        self.psw = es.enter_context(nc.psum_tensor("psw", [128, 1024], F32))
        self.ps += [self.psw[:, 0:512], self.psw[:, 512:1024]]
        self.psb = [mk.buf("ps%d" % i) for i in range(6)]
        self.pst2 = es.enter_context(nc.psum_tensor("pst2", [128, 1024], BF16))
        self.pst2b = mk.buf("pst2")
        self.pst = es.enter_context(nc.psum_tensor("pst", [128, 1024], BF16))
        self.pstb = mk.buf("pst")
        self.pstsb = [mk.buf("psts%d" % i) for i in range(8)]
        self.pstall = self.pstsb
        self.rr = 0

    def din(self, name, shape, dt=F32):
        self.inp[name] = self.nc.dram_tensor(name, list(shape), dt, kind="ExternalInput").ap()
        return self.inp[name]

    def dscr(self, name, shape, dt):
        kind = "ExternalOutput" if name in self.dbg else "Internal"
        self.scr[name] = self.nc.dram_tensor(name, list(shape), dt, kind=kind).ap()
        self.scrb[name] = self.mk.buf(name)
        if name in self.dbg:
            self.outs.append(self.scrb[name])
        return self.scr[name]

    def sb(self, stack, name, shape, dt):
        t = stack.enter_context(self.nc.sbuf_tensor(name, list(shape), dt))
        return t, self.mk.buf(name)

    def bank(self, n=4):
        i = self.rr % n
        self.rr += 1
        return self.ps[i], self.psb[i]

    def consts(self, st):
        mk, nc = self.mk, self.nc
        self.ident, self.identb_ = self.sb(st, "ident", [128, 128], F32)
        self.identh, self.identhb = self.sb(st, "identh", [128, 128], BF16)
        self.mut, self.mutb = self.sb(st, "mut", [64, 64], F32)
        self.mlt, self.mltb = self.sb(st, "mlt", [64, 64], F32)
        c = self.inp["consts"]
        mk.dma("sp", self.ident[:], c[:, 0:128], [], [self.identb_], self.identb_)
        mk.dma("sp", self.mut[:], c[0:64, 128:192], [], [self.mutb], self.mutb)
        mk.dma("sp", self.mlt[:], c[0:64, 192:256], [], [self.mltb], self.mltb)
        mk.op("dve", lambda e: e.tensor_copy(out=self.identh[:], in_=self.ident[:]), [self.identb_], [self.identhb])

    def transpose_to(self, src, srcb, dst_fn, dstb, nblk, bf):
        mk = self.mk
        if bf:
            per = 8
            for j0 in range(0, nblk, per):
                n = min(per, nblk - j0)
                for j in range(n):
                    mk.op("pe", lambda e, j=j: e.transpose(self.pst[:, j * 128:(j + 1) * 128], src[:, (j0 + j) * 128:(j0 + j + 1) * 128], self.identh[:]),
                          [srcb, self.identhb], self.pstsb, sig=(j == n - 1))
                mk.op("act", lambda e: e.copy(out=dst_fn(j0, n), in_=self.pst[:, 0:n * 128].rearrange("p (j t) -> p j t", t=128)),
                      self.pstsb, [dstb])
        else:
            per = 4
            k = 0
            for j0 in range(0, nblk, per):
                n = min(per, nblk - j0)
                pi = 4 + (k % 2)
                k += 1
                ps, psb = self.ps[pi], self.psb[pi]
                for j in range(n):
                    mk.op("pe", lambda e, j=j: e.transpose(ps[:, j * 128:(j + 1) * 128], src[:, (j0 + j) * 128:(j0 + j + 1) * 128], self.ident[:]),
                          [srcb, self.identb_], [psb], sig=(j == n - 1))
                eng = "act" if (k % 2) else "dve"
                if eng == "act":
                    mk.op("act", lambda e: e.copy(out=dst_fn(j0, n), in_=ps[:, 0:n * 128].rearrange("p (j t) -> p j t", t=128)), [psb], [dstb])
                else:
                    mk.op("dve", lambda e: e.tensor_copy(out=dst_fn(j0, n), in_=ps[:, 0:n * 128].rearrange("p (j t) -> p j t", t=128)), [psb], [dstb])

    def load_w_panel(self, wt, wb, W, c0, w, kc=KC):
        self.mk.dma("pool", wt[:, 0:kc, 0:w], W[:, c0:c0 + w].rearrange("(kc p) n -> p kc n", p=128), [], [wb], wb)

    def layer_norm(self, x, xb, st_t, st_b, w_bc, w_bcb, b_bc, b_bcb, n=128):
        mk = self.mk
        stats, mv, sd = st_t
        for j in range(4):
            mk.op("dve", lambda e, j=j: e.bn_stats(out=stats[0:n, j, :], in_=x[0:n, j * 512:(j + 1) * 512]), [xb], [st_b])
        mk.op("dve", lambda e: e.bn_aggr(out=mv[0:n, :], in_=stats[0:n, :, :].rearrange("p a b -> p (a b)")), [st_b], [st_b])
        mk.op("act", lambda e: e.activation(out=sd[0:n, :], in_=mv[0:n, 1:2], func=AF.Sqrt, bias=self.epsln[0:n, :], scale=1.0), [st_b, self.epsb], [st_b])
        mk.op("dve", lambda e: e.reciprocal(out=sd[0:n, :], in_=sd[0:n, :]), [st_b], [st_b])
        mk.op("dve", lambda e: e.tensor_scalar(out=x[0:n, :], in0=x[0:n, :], scalar1=mv[0:n, 0:1], scalar2=sd[0:n, 0:1], op0=ALU.subtract, op1=ALU.mult), [xb, st_b], [xb])
        mk.op("pool", lambda e: e.tensor_tensor(out=x[0:n, :], in0=x[0:n, :], in1=w_bc[0:n, :], op=ALU.mult), [xb, w_bcb], [xb])
        mk.op("dve", lambda e: e.tensor_tensor(out=x[0:n, :], in0=x[0:n, :], in1=b_bc[0:n, :], op=ALU.add), [xb, b_bcb], [xb])

    def proj(self, st, xT, xTb, ntok, W, groups, tok_off, wslots, stg):
        mk = self.mk
        wi = 0
        si = 0
        for g in groups:
            c0, w = g["c0"], g["w"]
            for p0 in range(0, w, 512):
                pw = min(512, w - p0)
                wt, wb = wslots[wi % 2]
                wi += 1
                self.load_w_panel(wt, wb, W, c0 + p0, pw)
                dst, dstb = self.scr[g["dst"]], self.scrb[g["dst"]]
                bf = (dst.dtype == BF16)
                if g["orient"] == "a":
                    for t0 in range(0, ntok, 128):
                        ps, psb = self.bank()
                        for kc in range(KC):
                            mk.op("pe", lambda e, kc=kc: e.matmul(ps[:, 0:pw], xT[:, kc, t0:t0 + 128], wt[:, kc, 0:pw], start=(kc == 0), stop=(kc == KC - 1)),
                                  [xTb, wb], [psb], sig=(kc == KC - 1))
                        sg, sgb = stg[bf][si % 4]
                        si += 1
                        self.evac(g, sg[:, 0:pw], ps[:, 0:pw], psb, sgb)
                        mk.dma("sp", dst[tok_off + t0:tok_off + t0 + 128, g["dc0"] + p0:g["dc0"] + p0 + pw], sg[:, 0:pw], [sgb], [dstb], sgb)
                else:
                    for j0 in range(0, pw, 128):
                        m = min(128, pw - j0)
                        for t0 in range(0, ntok, 512):
                            n = min(512, ntok - t0)
                            ps, psb = self.bank()
                            for kc in range(KC):
                                mk.op("pe", lambda e, kc=kc: e.matmul(ps[0:m, 0:n], wt[:, kc, j0:j0 + m], xT[:, kc, t0:t0 + n], start=(kc == 0), stop=(kc == KC - 1)),
                                      [xTb, wb], [psb], sig=(kc == KC - 1))
                            sg, sgb = stg[bf][si % 4]
                            si += 1
                            self.evac(g, sg[0:m, 0:n], ps[0:m, 0:n], psb, sgb)
                            r0 = g["dc0"] + p0 + j0
                            mk.dma("sp", dst[r0:r0 + m, tok_off + t0:tok_off + t0 + n], sg[0:m, 0:n], [sgb], [dstb], sgb)

    def evac(self, g, out, in_, inb, outb):
        mk = self.mk
        k = g["kind"]
        if k == "copy":
            self.ev = getattr(self, "ev", 0) + 1
            if self.ev % 2:
                mk.op("dve", lambda e: e.tensor_copy(out=out, in_=in_), [inb], [outb])
            else:
                mk.op("act", lambda e: e.copy(out=out, in_=in_), [inb], [outb])
        elif k == "scale":
            mk.op("act", lambda e: e.mul(out, in_, g["scale"]), [inb], [outb])
        elif k == "sigmoid":
            mk.op("act", lambda e: e.activation(out=out, in_=in_, func=AF.Sigmoid), [inb], [outb])
        elif k == "silu":
            mk.op("act", lambda e: e.activation(out=out, in_=in_, func=AF.Silu), [inb], [outb])
        else:
            raise ValueError(k)

    def build_xT(self, st, src, srcb_none, xT, xTb, tok_off, ntok, xin):
        mk = self.mk
        for i, t0 in enumerate(range(0, ntok, 128)):
            xt, xb = xin[i % 2]
            mk.dma("sp", xt[:], src[tok_off + t0:tok_off + t0 + 128, :], [srcb_none] if srcb_none else [], [xb], xb)
            self.transpose_to(xt, xb, lambda j0, n, t0=t0: xT[:, j0:j0 + n, t0:t0 + 128], xTb, KC, False)

    def l0_proj(self):
        mk, nc = self.mk, self.nc
        S = self.dscr
        S("QA_T", [512, F], BF16); S("KA_T", [512, T], BF16); S("VA", [T, 1024], BF16); S("OA", [F, 1024], BF16)
        S("GI_T", [8, T], F32); S("GF_T", [8, T], F32)
        S("QB_T", [1024, F], BF16); S("FF_T", [1024, F], F32); S("FB_T", [1024, T], F32)
        S("IB", [T, 1024], BF16); S("GB", [F, 1024], BF16)
        G = lambda c0, w, o, k, dst, dc0=0, scale=None: dict(c0=c0, w=w, orient=o, kind=k, dst=dst, dc0=dc0, scale=scale)
        g_ka = G(C_KA, 512, "b", "copy", "KA_T")
        g_va = G(C_VA, 1024, "a", "copy", "VA")
        g_gi = G(C_GI, 8, "b", "copy", "GI_T")
        g_gf = G(C_GF, 8, "b", "copy", "GF_T")
        g_fb = G(C_FB, 1024, "b", "copy", "FB_T")
        g_ib = G(C_IB, 1024, "a", "copy", "IB")
        full = [G(C_QA, 512, "b", "scale", "QA_T", scale=128 ** -0.5), g_ka, g_va, G(C_OA, 1024, "a", "sigmoid", "OA"),
                g_gi, g_gf, G(C_QB, 1024, "b", "silu", "QB_T"), G(C_FF, 1024, "b", "copy", "FF_T"), g_fb, g_ib,
                G(C_GB, 1024, "a", "silu", "GB")]
        state = [g_ka, g_va, g_gi, g_gf, g_fb, g_ib]
        with ExitStack() as st:
            xT, xTb = self.sb(st, "xT", [128, KC, F], BF16)
            xin = [self.sb(st, "xin%d" % i, [128, D], F32) for i in range(2)]
            wsl = [self.sb(st, "wp%d" % i, [128, KC, 512], BF16) for i in range(2)]
            stg = {False: [self.sb(st, "sg32_%d" % i, [128, 512], F32) for i in range(4)],
                   True: [self.sb(st, "sg16_%d" % i, [128, 512], BF16) for i in range(4)]}
            self.build_xT(st, self.inp["x"], None, xT, xTb, 0, F, xin)
            self.proj(st, xT, xTb, F, self.inp["w_in"], full, 0, wsl, stg)
            self.build_xT(st, self.inp["x"], None, xT, xTb, F, T - F, xin)
            self.proj(st, xT, xTb, T - F, self.inp["w_in"], state, F, wsl, stg)
            mk.barrier()


def make_consts():
    c = np.zeros((128, 512), np.float32)
    c[:, 256:384] = (np.arange(128)[:, None] < np.arange(128)[None, :])
    c[:, 384:512] = 1.0
    c[:, 0:128] = np.eye(128, dtype=np.float32)
    s = np.arange(64)[:, None]
    t = np.arange(64)[None, :]
    c[0:64, 128:192] = (s <= t)
    c[0:64, 192:256] = (s >= t)
    return c


def win_perm(flip):
    o = {}
    splits = [512, 512, 1024, 1024, 16, 1024, 1024, 1024, 1024, 1024]
    names = ["qa", "ka", "va", "oa", "g", "qb", "ff", "fb", "ib", "gb"]
    c = 0
    for n, s in zip(names, splits):
        o[n] = np.arange(c, c + s)
        c += s
    g = o["g"]
    i_f, f_f, i_b, f_b = g[0:4], g[4:8], g[8:12], g[12:16]
    if flip:
        i_f, f_f, i_b, f_b = i_b, f_b, i_f, f_f
        ff, fb = o["fb"], o["ff"]
    else:
        ff, fb = o["ff"], o["fb"]
    return np.concatenate([o["qa"], o["ka"], o["va"], o["oa"], i_f, i_b, f_f, f_b, o["qb"], ff, fb, o["ib"], o["gb"]])


def _p(cls):
    def deco(f):
        setattr(cls, f.__name__, f)
        return f
    return deco


@_p(Prog)
def small_consts(self, st):
    mk = self.mk
    self.one, self.oneb = self.sb(st, "one", [128, 1], F32)
    self.epsln, self.epsb = self.sb(st, "epsln", [128, 1], F32)
    self.epsrms, self.epsrb = self.sb(st, "epsrms", [128, 1], F32)
    mk.op("dve", lambda e: e.memset(self.one[:], 1.0), [], [self.oneb])
    mk.op("dve", lambda e: e.memset(self.epsln[:], LN_EPS), [], [self.epsb])
    mk.op("dve", lambda e: e.memset(self.epsrms[:], RMS_EPS), [], [self.epsrb])


@_p(Prog)
def l0_gates(self):
    mk = self.mk
    S = self.dscr
    S("EQ", [8, T], F32); S("EK", [8, T], F32); S("EE", [8, T], F32); S("EG", [8, NCT], F32)
    with ExitStack() as st:
        gi, gib = self.sb(st, "gi", [8, T], F32)
        gf, gfb = self.sb(st, "gf", [8, T], F32)
        b_, bb = self.sb(st, "gB", [8, T], F32)
        br, brb = self.sb(st, "gBR", [8, T], F32)
        tmp, tmpb = self.sb(st, "gtmp", [8, T], F32)
        rm, rmb = self.sb(st, "grm", [8, T], F32)
        gbias, gbb = self.sb(st, "gbias_s", [8, 4], F32)
        eg, egb = self.sb(st, "geg", [8, NCT], F32)
        mk.dma("sp", gi[:], self.scr["GI_T"], [self.scrb["GI_T"]], [gib], gib)
        mk.dma("sp", gf[:], self.scr["GF_T"], [self.scrb["GF_T"]], [gfb], gfb)
        mk.dma("sp", rm[:], self.inp["rmask"][0:8, 0:T], [], [rmb], rmb)
        mk.dma("sp", gbias[:, 0:3], self.inp["gbias"], [], [gbb], gbb)
        mk.op("dve", lambda e: e.tensor_scalar(out=gbias[:, 0:2], in0=gbias[:, 0:2], scalar1=1.0 / 15.0, scalar2=None, op0=ALU.mult), [gbb], [gbb])
        mk.op("act", lambda e: e.activation(out=gi[:], in_=gi[:], func=AF.Tanh, bias=gbias[:, 0:1], scale=1.0 / 15.0), [gib, gbb], [gib])
        mk.op("act", lambda e: e.activation(out=gf[:], in_=gf[:], func=AF.Tanh, bias=gbias[:, 1:2], scale=1.0 / 15.0), [gfb, gbb], [gfb])
        mk.op("dve", lambda e: e.tensor_scalar(out=gi[:], in0=gi[:], scalar1=15.0, scalar2=None, op0=ALU.mult), [gib], [gib])
        mk.op("act", lambda e: e.activation(out=gf[:], in_=gf[:], func=AF.Exp, scale=-15.0), [gfb], [gfb])
        mk.op("act", lambda e: e.activation(out=gf[:], in_=gf[:], func=AF.Ln, bias=self.one[0:8, :], scale=1.0), [gfb, self.oneb], [gfb])
        mk.op("dve", lambda e: e.tensor_scalar(out=gf[:], in0=gf[:], scalar1=-1.0, scalar2=None, op0=ALU.mult), [gfb], [gfb])
        mk.op("dve", lambda e: e.tensor_tensor_scan(out=b_[:], data0=rm[:], data1=gf[:], initial=0.0, op0=ALU.mult, op1=ALU.add), [rmb, gfb], [bb])
        v3 = lambda t: t[:].rearrange("p (c l) -> p c l", l=CH)
        mk.op("dve", lambda e: e.tensor_copy(out=eg[:].rearrange("p (c o) -> p c o", o=1), in_=v3(b_)[:, :, CH - 1:CH]), [bb], [egb])
        gbc = eg[:].rearrange("p (c o) -> p c o", o=1).to_broadcast([8, NCT, CH])
        mk.op("dve", lambda e: e.tensor_tensor(out=v3(br), in0=gbc, in1=v3(b_), op=ALU.subtract), [egb, bb], [brb])
        mk.op("dve", lambda e: e.tensor_tensor(out=br[:], in0=br[:], in1=gf[:], op=ALU.add), [brb, gfb], [brb])
        mk.op("dve", lambda e: e.tensor_tensor(out=tmp[:], in0=b_[:], in1=br[:], op=ALU.subtract), [bb, brb], [tmpb])
        mk.op("dve", lambda e: e.scalar_tensor_tensor(out=b_[:], in0=tmp[:], scalar=gbias[:, 2:3], in1=br[:], op0=ALU.mult, op1=ALU.add), [tmpb, brb, gbb], [bb])
        mk.op("act", lambda e: e.activation(out=tmp[:], in_=b_[:], func=AF.Exp), [bb], [tmpb])
        mk.dma("sp", self.scr["EQ"], tmp[:], [tmpb], [self.scrb["EQ"]], tmpb)
        mk.op("dve", lambda e: e.tensor_tensor(out=br[:], in0=gi[:], in1=b_[:], op=ALU.subtract), [gib, bb], [brb])
        mk.op("act", lambda e: e.activation(out=br[:], in_=br[:], func=AF.Exp), [brb], [brb])
        mk.dma("sp", self.scr["EK"], br[:], [brb], [self.scrb["EK"]], brb)
        mk.op("act", lambda e: e.activation(out=eg[:], in_=eg[:], func=AF.Exp), [egb], [egb])
        mk.dma("sp", self.scr["EG"], eg[:], [egb], [self.scrb["EG"]], egb)
        mk.op("dve", lambda e: e.tensor_tensor(out=v3(gi), in0=v3(br), in1=gbc, op=ALU.mult), [brb, egb], [gib])
        mk.dma("sp", self.scr["EE"], gi[:], [gib], [self.scrb["EE"]], gib)
        mk.barrier()


@_p(Prog)
def rec_pass1(self, chains, V, Vb, dvp):
    mk = self.mk
    GP = 8
    for ci, ch in enumerate(chains):
        mk.op("dve", lambda e: e.memset(ch["S"][:, 0:dvp], 0.0), [], [ch["Sb"]])
        ch["groups"] = [ch["order"][k:k + GP] for k in range(0, len(ch["order"]), GP)]
        ch["bank"] = (self.pst, self.pstall) if ci == 0 else (self.pst2, [self.pst2b])
    ng = max(len(ch["groups"]) for ch in chains)
    for g in range(ng + 1):
        for ch in chains:
            if g < len(ch["groups"]):
                grp = ch["groups"][g]
                bk, bkb = ch["bank"]
                for k, c in enumerate(grp):
                    mk.op("pe", lambda e: e.transpose(bk[0:CH, k * 128:(k + 1) * 128], ch["kend"][:, c * CH:(c + 1) * CH], self.identh[:]),
                          [ch["kendb"], self.identhb], bkb, sig=(k == len(grp) - 1))
                kt, ktb = ch["kts"][g % 2]
                n = len(grp)
                mk.op("act", lambda e: e.copy(out=kt[:, 0:n, :], in_=bk[0:CH, 0:n * 128].rearrange("p (j t) -> p j t", t=128)), bkb, [ktb])
        if g >= 1:
            for k in range(GP):
                for ch in chains:
                    if g - 1 < len(ch["groups"]) and k < len(ch["groups"][g - 1]):
                        c = ch["groups"][g - 1][k]
                        kt, ktb = ch["kts"][(g - 1) % 2]
                        ps, psb = self.bank(NBK)
                        S, Sb = ch["S"], ch["Sb"]
                        mk.op("pe", lambda e: e.matmul(ps[:, 0:dvp], kt[:, k, :], V[:, c, 0:dvp], start=True, stop=True), [ktb, Vb], [psb])
                        mk.op("dve", lambda e: e.scalar_tensor_tensor(out=S[:, 0:dvp], in0=S[:, 0:dvp], scalar=ch["decay"][:, c:c + 1], in1=ps[:, 0:dvp], op0=ALU.mult, op1=ALU.add),
                              [Sb, ch["decayb"], psb], [Sb])
                        s_ = ch["store"](c)
                        if s_ is not None:
                            mk.op("act", lambda e: e.copy(out=ch["Sbf"][:, s_, 0:dvp], in_=S[:, 0:dvp]), [Sb], [ch["Sbfb"]])


@_p(Prog)
def rec_scores2(self, c, kin, qin, masks, pts, i):
    mk = self.mk
    sl = slice(c * CH, (c + 1) * CH)
    out = []
    for d in range(2):
        ps, psb = self.bank(NBK)
        mk.op("pe", lambda e: e.matmul(ps[0:CH, 0:CH], kin[d][0][:, sl], qin[d][0][:, sl], start=True, stop=True), [kin[d][1], qin[d][1]], [psb])
        pt, ptb = pts[(2 * i + d) % len(pts)]
        mk.op("dve", lambda e: e.tensor_tensor(out=pt[:], in0=ps[0:CH, 0:CH], in1=masks[d][0][:], op=ALU.mult), [psb, masks[d][1]], [ptb])
        out.append((pt, ptb))
    return out


@_p(Prog)
def l0_mlstm(self):
    mk = self.mk
    if "Y" not in self.scr:
        self.dscr("Y", [F, D], BF16)
    Y, Yb = self.scr["Y"], self.scrb["Y"]
    dvp = 257
    with ExitStack() as st:
        qraw, qrawb = self.sb(st, "m_qraw", [128, F], BF16)
        kraw, krawb = self.sb(st, "m_kraw", [128, T], BF16)
        bc, bcb = self.sb(st, "m_bc", [128, T], F32)
        qin = [self.sb(st, "m_qin%d" % d, [128, F], BF16) for d in range(2)]
        kin = [self.sb(st, "m_kin%d" % d, [128, F], BF16) for d in range(2)]
        kend = [self.sb(st, "m_kend0", [128, F], BF16), self.sb(st, "m_kend1", [128, T], BF16)]
        decay = [self.sb(st, "m_dec%d" % d, [128, NCT], F32) for d in range(2)]
        V, Vb = self.sb(st, "m_V", [CH, NCT, dvp], BF16)
        Sbf = [self.sb(st, "m_Sbf%d" % d, [128, NCF + 1, dvp], BF16) for d in range(2)]
        S2 = [self.sb(st, "m_S%d" % i, [128, dvp], F32) for i in range(2)]
        kts = [self.sb(st, "m_kt%d" % i, [CH, 8, 128], BF16) for i in range(4)]
        pts = [self.sb(st, "m_pt%d" % i, [CH, CH], BF16) for i in range(8)]
        oaw, oawb = self.sb(st, "m_oaw", [CH, NCF, 256], BF16)
        wbc, wbcb = self.sb(st, "m_wbc", [CH, 256], F32)
        Yh, Yhb = self.sb(st, "m_Yh", [CH, NCF, 256], BF16)
        hs = [self.sb(st, "m_hs%d" % i, [CH, 256], F32) for i in range(3)]
        junk, junkb = self.sb(st, "m_junk", [CH, 256], F32)
        sm = [self.sb(st, "m_sm%d" % i, [CH, 8], F32) for i in range(4)]
        masks = [(self.mut, self.mutb), (self.mlt, self.mltb)]
        for h in range(4):
            mk.dma("sp", qraw[:], self.scr["QA_T"][h * 128:(h + 1) * 128, :], [self.scrb["QA_T"]], [qrawb], qrawb)
            mk.dma("sp", kraw[:], self.scr["KA_T"][h * 128:(h + 1) * 128, :], [self.scrb["KA_T"]], [krawb], krawb)
            mk.dma("sp", V[:, :, 0:256], self.scr["VA"][:, h * 256:(h + 1) * 256].rearrange("(c p) e -> p c e", p=CH), [self.scrb["VA"]], [Vb], Vb)
            mk.op("dve", lambda e: e.memset(V[:, :, 256:257], 1.0), [], [Vb])
            mk.dma("sp", oaw[:], self.scr["OA"][:, h * 256:(h + 1) * 256].rearrange("(c p) e -> p c e", p=CH), [self.scrb["OA"]], [oawb], oawb)
            mk.dma("sp", wbc[:], self.inp["mlstm_norm_w"][h:h + 1, :].to_broadcast([CH, 256]), [], [wbcb], wbcb)
            mk.op("dve", lambda e: e.tensor_tensor(out=oaw[:], in0=oaw[:], in1=wbc[:].rearrange("p (o e) -> p o e", o=1).to_broadcast([CH, NCF, 256]), op=ALU.mult), [oawb, wbcb], [oawb])
            for d in range(2):
                r = d * 4 + h
                Td = F if d == 0 else T
                for (name, dst, src, n) in (("EQ", qin[d], qraw, F), ("EK", kin[d], kraw, F), ("EE", kend[d], kraw, Td)):
                    mk.dma("sp", bc[:, 0:n], self.scr[name][r:r + 1, 0:n].to_broadcast([128, n]), [self.scrb[name]], [bcb], bcb)
                    mk.op("dve", lambda e, dst=dst, src=src, n=n: e.tensor_tensor(out=dst[0][:, 0:n], in0=src[:, 0:n], in1=bc[:, 0:n], op=ALU.mult),
                          [bcb, qrawb, krawb], [dst[1]])
                mk.dma("sp", decay[d][0][:], self.scr["EG"][r:r + 1, :].to_broadcast([128, NCT]), [self.scrb["EG"]], [decay[d][1]], decay[d][1])
            chains = [dict(kend=kend[0][0], kendb=kend[0][1], decay=decay[0][0], decayb=decay[0][1], S=S2[0][0], Sb=S2[0][1], Sbf=Sbf[0][0], Sbfb=Sbf[0][1],
                           kts=kts[0:2], order=list(range(NCF - 1)), store=lambda c: c + 1, slot0=0),
                      dict(kend=kend[1][0], kendb=kend[1][1], decay=decay[1][0], decayb=decay[1][1], S=S2[1][0], Sb=S2[1][1], Sbf=Sbf[1][0], Sbfb=Sbf[1][1],
                           kts=kts[2:4], order=list(range(NCT - 1, 0, -1)), store=lambda c: (c - 1) if c <= NCF else None, slot0=4)]
            self.rec_pass1(chains, V, Vb, dvp)
            stA, stO = {}, {}
            for it in range(NCF + SK_E):
                c = it
                if c < NCF:
                    stA[c] = self.rec_scores2(c, kin, qin, masks, pts, c)
                c = it - SK_O
                if 0 <= c < NCF:
                    sl = slice(c * CH, (c + 1) * CH)
                    pso = []
                    for d in range(2):
                        pt, ptb = stA[c][d]
                        ps, psb = self.bank(NBK)
                        has_state = (c > 0) if d == 0 else True
                        mk.op("pe", lambda e: e.matmul(ps[0:CH, 0:dvp], pt[:], V[:, c, :], start=True, stop=not has_state), [ptb, Vb], [psb], sig=not has_state)
                        if has_state:
                            mk.op("pe", lambda e: e.matmul(ps[0:CH, 0:dvp], qin[d][0][:, sl], Sbf[d][0][:, c, :], start=False, stop=True), [qin[d][1], Sbf[d][1]], [psb])
                        pso.append((ps, psb))
                    hsT, hsb = hs[c % 3]
                    s_, s_b = sm[c % 4]
                    for d in range(2):
                        ps, psb = pso[d]
                        o = 4 * d
                        mk.op("dve", lambda e: e.tensor_scalar(out=s_[:, o + 3:o + 4], in0=ps[0:CH, 256:257], scalar1=-1.0, scalar2=1.0, op0=ALU.mult, op1=ALU.max), [psb], [s_b])
                        mk.op("dve", lambda e: e.scalar_tensor_tensor(out=s_[:, o:o + 1], in0=ps[0:CH, 256:257], scalar=1.0, in1=s_[:, o + 3:o + 4], op0=ALU.max, op1=ALU.max), [psb, s_b], [s_b])
                        mk.op("dve", lambda e: e.reciprocal(out=s_[:, o:o + 1], in_=s_[:, o:o + 1]), [s_b], [s_b])
                        if d == 0:
                            mk.op("act", lambda e: e.activation(out=hsT[:], in_=ps[0:CH, 0:256], func=AF.Copy, scale=s_[:, o:o + 1]), [psb, s_b], [hsb])
                        else:
                            mk.op("dve", lambda e: e.scalar_tensor_tensor(out=hsT[:], in0=ps[0:CH, 0:256], scalar=s_[:, o:o + 1], in1=hsT[:], op0=ALU.mult, op1=ALU.add), [psb, s_b, hsb], [hsb])
                    mk.op("act", lambda e: e.activation(out=junk[:], in_=hsT[:], func=AF.Square, accum_out=s_[:, 1:2]), [hsb], [junkb, s_b])
                    mk.op("act", lambda e: e.activation(out=s_[:, 2:3], in_=s_[:, 1:2], func=AF.Sqrt, bias=self.epsrms[0:CH, :], scale=1.0 / 256.0), [s_b, self.epsrb], [s_b])
                c = it - SK_E
                if 0 <= c < NCF:
                    hsT, hsb = hs[c % 3]
                    s_, s_b = sm[c % 4]
                    mk.op("dve", lambda e: e.reciprocal(out=s_[:, 2:3], in_=s_[:, 2:3]), [s_b], [s_b])
                    mk.op("dve", lambda e: e.scalar_tensor_tensor(out=Yh[:, c, :], in0=hsT[:], scalar=s_[:, 2:3], in1=oaw[:, c, :], op0=ALU.mult, op1=ALU.mult), [hsb, s_b, oawb], [Yhb])
            mk.dma("sp", Y[:, h * 256:(h + 1) * 256].rearrange("(c p) e -> p c e", p=CH), Yh[:], [Yhb], [Yb], Yhb)
        mk.barrier()


@_p(Prog)
def l0_hgrn(self):
    mk = self.mk
    if "Y" not in self.scr:
        self.dscr("Y", [F, D], BF16)
    Y, Yb = self.scr["Y"], self.scrb["Y"]
    dvp = 128
    v3 = lambda ap: ap.rearrange("p (c l) -> p c l", l=CH)
    REV = False
    with ExitStack() as st:
        A, Ab = self.sb(st, "h_A", [128, T], F32)
        B, Bb = self.sb(st, "h_B", [128, T], F32)
        C, Cb = self.sb(st, "h_C", [128, T], F32)
        rm, rmb = self.sb(st, "h_rm", [128, T + CH], F32)
        Abq = [mk.buf("h_Aq%d" % i) for i in range(4)]
        Bbq = [mk.buf("h_Bq%d" % i) for i in range(4)]
        Cbq = [mk.buf("h_Cq%d" % i) for i in range(4)]
        qs, qsb = self.sb(st, "h_qs", [128, F], BF16)
        qin = [self.sb(st, "h_qin%d" % d, [128, F], BF16) for d in range(2)]
        kin = [self.sb(st, "h_kin0", [128, F], BF16), self.sb(st, "h_kin1", [128, T], BF16)]
        kend = [self.sb(st, "h_kend0", [128, F], BF16), self.sb(st, "h_kend1", [128, T], BF16)]
        decay = [self.sb(st, "h_dec%d" % d, [128, NCT], F32) for d in range(2)]
        V, Vb = self.sb(st, "h_V", [CH, NCT, dvp], BF16)
        Sbf = [self.sb(st, "h_Sbf%d" % d, [128, NCF + 1, dvp], BF16) for d in range(2)]
        S2 = [self.sb(st, "h_S%d" % i, [128, dvp], F32) for i in range(2)]
        kts = [self.sb(st, "h_kt%d" % i, [CH, 8, 128], BF16) for i in range(4)]
        pts = [self.sb(st, "h_pt%d" % i, [CH, CH], BF16) for i in range(8)]
        gbw, gbwb = self.sb(st, "h_gbw", [CH, NCF, 128], BF16)
        wbc, wbcb = self.sb(st, "h_wbc", [CH, 128], F32)
        Yhs = [self.sb(st, "h_Yh%d" % i, [CH, NCF, 128], BF16) for i in range(2)]
        junk, junkb = self.sb(st, "h_junk", [CH, 128], F32)
        sm = [self.sb(st, "h_sm%d" % i, [CH, 4], F32) for i in range(4)]
        lbl, lblb = self.sb(st, "h_lbl", [128, 32], F32)
        LB, LBb = self.sb(st, "h_LB", [128, 16], F32)
        OML, OMLb = self.sb(st, "h_OML", [128, 16], F32)
        masks = [(self.mut, self.mutb), (self.mlt, self.mltb)]
        mk.dma("sp", rm[:], self.inp["rmask"], [], [rmb], rmb)
        mk.dma("sp", lbl[:], self.inp["lbl"], [], [lblb], lblb)
        l4 = lbl[:].rearrange("p (d s h) -> p d s h", d=2, s=2)
        mk.op("dve", lambda e: e.tensor_tensor(out=LB[:].rearrange("p (d h) -> p d h", d=2), in0=l4[:, :, 0, :], in1=l4[:, :, 1, :], op=ALU.subtract), [lblb], [LBb])
        mk.op("act", lambda e: e.activation(out=LB[:], in_=LB[:], func=AF.Sigmoid), [LBb], [LBb])
        mk.op("dve", lambda e: e.tensor_scalar(out=OML[:], in0=LB[:], scalar1=-1.0, scalar2=1.0, op0=ALU.mult, op1=ALU.add), [LBb], [OMLb])
        for h in range(8):
            rows = slice(h * 128, (h + 1) * 128)
            Yh, Yhb = Yhs[h % 2]
            mk.dma("sp", qs[:], self.scr["QB_T"][rows, :], [self.scrb["QB_T"]], [qsb], qsb)
            mk.dma("sp", V[:], self.scr["IB"][:, rows].rearrange("(c p) e -> p c e", p=CH), [self.scrb["IB"]], [Vb], Vb)
            mk.dma("sp", gbw[:], self.scr["GB"][:, rows].rearrange("(c p) e -> p c e", p=CH), [self.scrb["GB"]], [gbwb], gbwb)
            mk.dma("sp", wbc[:], self.inp["hgrn_norm_w"][h:h + 1, :].to_broadcast([CH, 128]), [], [wbcb], wbcb)
            mk.op("pool", lambda e: e.tensor_tensor(out=gbw[:], in0=gbw[:], in1=wbc[:].rearrange("p (o e) -> p o e", o=1).to_broadcast([CH, NCF, 128]), op=ALU.mult), [gbwb, wbcb], [gbwb])
            for d in range(2):
                Td = F if d == 0 else T
                nc_ = Td // CH
                src = "FF_T" if d == 0 else "FB_T"
                col = d * 8 + h
                omc, lbc = OML[:, col:col + 1], LB[:, col:col + 1]
                dec, decb = decay[d]
                blks = [(k, k * 1024, min((k + 1) * 1024, Td)) for k in range(4) if k * 1024 < Td]
                for k, s0, e0 in blks:
                    mk.dma("sp", A[:, s0:e0], self.scr[src][rows, s0:e0], [self.scrb[src]], [Abq[k]], Abq[k])
                for k, s0, e0 in blks:
                    mk.op("act", lambda e: e.activation(out=B[:, s0:e0], in_=A[:, s0:e0], func=AF.Sigmoid), [Abq[k]], [Bbq[k]])
                for k, s0, e0 in blks:
                    mk.op("act", lambda e: e.activation(out=A[:, s0:e0], in_=A[:, s0:e0], func=AF.Sigmoid, scale=-1.0), [Abq[k]], [Abq[k]])
                for k, s0, e0 in blks:
                    mk.op("act", lambda e: e.activation(out=B[:, s0:e0], in_=B[:, s0:e0], func=AF.Ln, scale=omc, bias=lbc), [Bbq[k], OMLb, LBb], [Bbq[k]])
                for k, s0, e0 in blks:
                    c0, c1 = s0 // CH, e0 // CH
                    mk.op("dve", lambda e: e.tensor_tensor_scan(out=C[:, s0:e0], data0=rm[:, s0:e0], data1=B[:, s0:e0], initial=0.0, op0=ALU.mult, op1=ALU.add), [rmb, Bbq[k]], [Cbq[k]])
                    mk.op("dve", lambda e: e.tensor_copy(out=dec[:, c0:c1].rearrange("p (c o) -> p c o", o=1), in_=v3(C[:, s0:e0])[:, :, CH - 1:CH]), [Cbq[k]], [decb])
                    if d == 1:
                        gb_ = dec[:, c0:c1].rearrange("p (c o) -> p c o", o=1).to_broadcast([128, c1 - c0, CH])
                        mk.op("dve", lambda e: e.scalar_tensor_tensor(out=v3(C[:, s0:e0]), in0=v3(C[:, s0:e0]), scalar=-1.0, in1=gb_, op0=ALU.mult, op1=ALU.add), [Cbq[k], decb], [Cbq[k]])
                        mk.op("pool", lambda e: e.tensor_tensor(out=C[:, s0:e0], in0=C[:, s0:e0], in1=B[:, s0:e0], op=ALU.add), [Cbq[k], Bbq[k]], [Cbq[k]])
                for k, s0, e0 in blks:
                    mk.op("act", lambda e: e.activation(out=B[:, s0:e0], in_=C[:, s0:e0], func=AF.Exp, scale=-1.0), [Cbq[k], Bbq[k]], [Bbq[k]])
                mk.op("act", lambda e: e.activation(out=dec[:, 0:nc_], in_=dec[:, 0:nc_], func=AF.Exp), [decb], [decb])
                for k, s0, e0 in blks:
                    c0, c1 = s0 // CH, e0 // CH
                    mk.op("dve", lambda e: e.scalar_tensor_tensor(out=kin[d][0][:, s0:e0], in0=A[:, s0:e0], scalar=omc, in1=B[:, s0:e0], op0=ALU.mult, op1=ALU.mult), [Abq[k], Bbq[k], OMLb], [kin[d][1]])
                    egb = dec[:, c0:c1].rearrange("p (c o) -> p c o", o=1).to_broadcast([128, c1 - c0, CH])
                    mk.op("pool", lambda e: e.tensor_tensor(out=v3(kend[d][0][:, s0:e0]), in0=v3(kin[d][0][:, s0:e0]), in1=egb, op=ALU.mult), [kin[d][1], decb], [kend[d][1]])
                for k, s0, e0 in blks:
                    e1 = min(e0, F)
                    if s0 < e1:
                        mk.op("act", lambda e: e.activation(out=A[:, s0:e1], in_=C[:, s0:e1], func=AF.Exp), [Cbq[k], Abq[k]], [Abq[k]])
                        mk.op("dve", lambda e: e.tensor_tensor(out=qin[d][0][:, s0:e1], in0=qs[:, s0:e1], in1=A[:, s0:e1], op=ALU.mult), [qsb, Abq[k]], [qin[d][1]])
            chains = [dict(kend=kend[0][0], kendb=kend[0][1], decay=decay[0][0], decayb=decay[0][1], S=S2[0][0], Sb=S2[0][1], Sbf=Sbf[0][0], Sbfb=Sbf[0][1],
                           kts=kts[0:2], order=list(range(NCF - 1)), store=lambda c: c + 1, slot0=0),
                      dict(kend=kend[1][0], kendb=kend[1][1], decay=decay[1][0], decayb=decay[1][1], S=S2[1][0], Sb=S2[1][1], Sbf=Sbf[1][0], Sbfb=Sbf[1][1],
                           kts=kts[2:4], order=list(range(NCT - 1, 0, -1)), store=lambda c: (c - 1) if c <= NCF else None, slot0=4)]
            self.rec_pass1(chains, V, Vb, dvp)
            stA, stO = {}, {}
            for it in range(NCF + SK_E):
                c = it
                if c < NCF:
                    stA[c] = self.rec_scores2(c, kin, qin, masks, pts, c)
                c = it - SK_O
                if 0 <= c < NCF:
                    sl = slice(c * CH, (c + 1) * CH)
                    ptl = stA[c]
                    ps, psb = self.bank(NBK)
                    mk.op("pe", lambda e: e.matmul(ps[0:CH, 0:dvp], ptl[0][0][:], V[:, c, :], start=True, stop=False), [ptl[0][1], Vb], [psb], sig=False)
                    mk.op("pe", lambda e: e.matmul(ps[0:CH, 0:dvp], ptl[1][0][:], V[:, c, :], start=False, stop=False), [ptl[1][1], Vb], [psb], sig=False)
                    if c > 0:
                        mk.op("pe", lambda e: e.matmul(ps[0:CH, 0:dvp], qin[0][0][:, sl], Sbf[0][0][:, c, :], start=False, stop=False), [qin[0][1], Sbf[0][1]], [psb], sig=False)
                    mk.op("pe", lambda e: e.matmul(ps[0:CH, 0:dvp], qin[1][0][:, sl], Sbf[1][0][:, c, :], start=False, stop=True), [qin[1][1], Sbf[1][1]], [psb])
                    stO[c] = (ps, psb)
                    s_, s_b = sm[c % 4]
                    mk.op("act", lambda e: e.activation(out=junk[:], in_=ps[0:CH, 0:dvp], func=AF.Square, accum_out=s_[:, 1:2]), [psb], [junkb, s_b])
                    mk.op("act", lambda e: e.activation(out=s_[:, 2:3], in_=s_[:, 1:2], func=AF.Sqrt, bias=self.epsrms[0:CH, :], scale=1.0 / 128.0), [s_b, self.epsrb], [s_b])
                c = it - SK_E
                if 0 <= c < NCF:
                    ps, psb = stO[c]
                    s_, s_b = sm[c % 4]
                    mk.op("dve", lambda e: e.reciprocal(out=s_[:, 2:3], in_=s_[:, 2:3]), [s_b], [s_b])
                    mk.op("dve", lambda e: e.scalar_tensor_tensor(out=Yh[:, c, :], in0=ps[0:CH, 0:dvp], scalar=s_[:, 2:3], in1=gbw[:, c, :], op0=ALU.mult, op1=ALU.mult), [psb, s_b, gbwb], [Yhb])
            mk.dma("pool", Y[:, 1024 + h * 128:1024 + (h + 1) * 128].rearrange("(c p) e -> p c e", p=CH), Yh[:], [Yhb], [Yb], Yhb)
        mk.barrier()


def make_rmask():
    r = np.ones((128, T + CH), np.float32)
    r[:, ::CH] = 0.0
    return r


def make_gbias(gb, flip):
    i_f, f_f, i_b, f_b = gb[0], gb[1], gb[2], gb[3]
    if flip:
        i_f, f_f, i_b, f_b = i_b, f_b, i_f, f_f
    out = np.zeros((8, 3), np.float32)
    out[:, 0] = np.concatenate([i_f, i_b])
    out[:, 1] = np.concatenate([f_f, f_b])
    out[0:4, 2] = 1.0
    return out


def make_lbl(lg, flip):
    a = lg[::-1] if flip else lg
    a = a.reshape(2, 2, 8, 128).transpose(3, 0, 1, 2).reshape(128, 32)
    return np.ascontiguousarray(a)


@_p(Prog)
def load_ln(self, st, i, k, tag):
    w_bc, w_bcb = self.sb(st, tag + "_lnw", [128, D], F32)
    b_bc, b_bcb = self.sb(st, tag + "_lnb", [128, D], F32)
    self.mk.dma("sp", w_bc[:], self.inp["ln_w"][2 * i + k:2 * i + k + 1, :].to_broadcast([128, D]), [], [w_bcb], w_bcb)
    self.mk.dma("sp", b_bc[:], self.inp["ln_b"][2 * i + k:2 * i + k + 1, :].to_broadcast([128, D]), [], [b_bcb], b_bcb)
    stats = self.sb(st, tag + "_st", [128, 4, 6], F32)
    mv = self.sb(st, tag + "_mv", [128, 2], F32)
    sd = self.sb(st, tag + "_sd", [128, 1], F32)
    return (w_bc, w_bcb, b_bc, b_bcb, (stats[0], mv[0], sd[0]), stats[1])


@_p(Prog)
def preload_w(self, st, tag, W):
    wres, wresb = self.sb(st, tag + "_w", [128, KC, D], BF16)
    for c0 in range(0, D, 512):
        self.mk.dma("pool", wres[:, :, c0:c0 + 512], W[:, c0:c0 + 512].rearrange("(kc p) n -> p kc n", p=128), [], [wresb], wresb)
    return wres, wresb


@_p(Prog)
def mix_ln(self, tag, ysrc, W, resid, residb, li, ntok, dst, pre=None):
    mk = self.mk
    Ys, Ysb = self.scr[ysrc], self.scrb[ysrc]
    with ExitStack() as st:
        wres, wresb = pre if pre is not None else self.preload_w(st, tag, W)
        w_bc, w_bcb, b_bc, b_bcb, stt, stb = self.load_ln(st, li, 0, tag)
        yt = [self.sb(st, tag + "_y%d" % i, [128, D], BF16) for i in range(3)]
        yT = [self.sb(st, tag + "_yT%d" % i, [128, KC, 128], BF16) for i in range(3)]
        xr = [self.sb(st, tag + "_x%d" % i, [128, D], F32) for i in range(3)]
        def loads(i):
            t0 = i * 128
            mk.dma("sp", yt[i % 3][0][:], Ys[t0:t0 + 128, :], [Ysb], [yt[i % 3][1]], yt[i % 3][1])
            mk.dma("sp", xr[i % 3][0][:], resid[t0:t0 + 128, :], [residb] if residb else [], [xr[i % 3][1]], xr[i % 3][1])
        loads(0)
        for i, t0 in enumerate(range(0, ntok, 128)):
            y, yb = yt[i % 3]
            yTt, yTb = yT[i % 3]
            x, xb = xr[i % 3]
            if t0 + 128 < ntok:
                loads(i + 1)
            self.transpose_to(y, yb, lambda j0, n: yTt[:, j0:j0 + n, :], yTb, KC, True)
            for cb in range(4):
                ps, psb = self.bank()
                for kc in range(KC):
                    mk.op("pe", lambda e: e.matmul(ps[:, :], yTt[:, kc, :], wres[:, kc, cb * 512:(cb + 1) * 512], start=(kc == 0), stop=(kc == KC - 1)),
                          [yTb, wresb], [psb], sig=(kc == KC - 1))
                mk.op("dve", lambda e: e.scalar_tensor_tensor(out=x[:, cb * 512:(cb + 1) * 512], in0=x[:, cb * 512:(cb + 1) * 512], scalar=ALPHA, in1=ps[:, :], op0=ALU.mult, op1=ALU.add),
                      [xb, psb], [xb])
            self.layer_norm(x, xb, stt, stb, w_bc, w_bcb, b_bc, b_bcb)
            mk.dma("sp", self.scr[dst][t0:t0 + 128, :], x[:], [xb], [self.scrb[dst]], xb)
        mk.barrier()


@_p(Prog)
def ffn_dense(self, src, ntok, dst):
    mk = self.mk
    Wg, Wu, Wd = self.inp["ffn_w_gate"], self.inp["ffn_w_up"], self.inp["ffn_w_down"]
    NH = FFN // 128
    TS = 768
    NB = 384
    with ExitStack() as st:
        xaT, xaTb = self.sb(st, "f_xT", [128, KC, TS], BF16)
        hT, hTb = self.sb(st, "f_hT", [128, NH, TS], BF16)
        acc = [self.sb(st, "f_acc%d" % i, [128, D], F32) for i in range(TS // 128)]
        wg = [self.sb(st, "f_wg%d" % i, [128, KC, 128], BF16) for i in range(2)]
        wu = [self.sb(st, "f_wu%d" % i, [128, KC, 128], BF16) for i in range(2)]
        wd = [self.sb(st, "f_wd%d" % i, [128, 8, 512], BF16) for i in range(2)]
        sg = [self.sb(st, "f_sg%d" % i, [128, NB], F32) for i in range(2)]
        na = 0
        for s0 in range(0, ntok, TS):
            n = min(TS, ntok - s0)
            ntile = n // 128
            for tt in range(ntile):
                a, ab = acc[tt]
                mk.dma("sp", a[:], self.scr[src][s0 + tt * 128:s0 + (tt + 1) * 128, :], [self.scrb[src]], [ab], ab)
                self.transpose_to(a, ab, lambda j0, m, tt=tt: xaT[:, j0:j0 + m, tt * 128:(tt + 1) * 128], xaTb, KC, False)
                mk.op("pool", lambda e: e.tensor_scalar(out=a[:], in0=a[:], scalar1=ALPHA, scalar2=None, op0=ALU.mult), [ab], [ab])
            for j in range(NH):
                g_, gb_ = wg[j % 2]
                u_, ub_ = wu[j % 2]
                self.load_w_panel(g_, gb_, Wg, j * 128, 128)
                self.load_w_panel(u_, ub_, Wu, j * 128, 128)
                for b0 in range(0, n, NB):
                    nb = min(NB, n - b0)
                    bs = slice(b0, b0 + nb)
                    psg, psgb = self.bank()
                    psu, psub = self.bank()
                    for kc in range(KC):
                        mk.op("pe", lambda e: e.matmul(psg[:, 0:nb], g_[:, kc, :], xaT[:, kc, bs], start=(kc == 0), stop=(kc == KC - 1)), [gb_, xaTb], [psgb], sig=(kc == KC - 1))
                    for kc in range(KC):
                        mk.op("pe", lambda e: e.matmul(psu[:, 0:nb], u_[:, kc, :], xaT[:, kc, bs], start=(kc == 0), stop=(kc == KC - 1)), [ub_, xaTb], [psub], sig=(kc == KC - 1))
                    s_, s_b = sg[(b0 // NB) % 2]
                    mk.op("act", lambda e: e.activation(out=s_[:, 0:nb], in_=psg[:, 0:nb], func=AF.Silu), [psgb], [s_b])
                    mk.op("dve", lambda e: e.tensor_tensor(out=hT[:, j, bs], in0=s_[:, 0:nb], in1=psu[:, 0:nb], op=ALU.mult), [s_b, psub], [hTb])
            pi = 0
            for cb in range(4):
                cs = slice(cb * 512, (cb + 1) * 512)
                for j0 in range(0, NH, 8):
                    nj = min(8, NH - j0)
                    d_, db_ = wd[pi % 2]
                    pi += 1
                    mk.dma("pool", d_[:, 0:nj, :], Wd[j0 * 128:(j0 + nj) * 128, cs].rearrange("(j p) n -> p j n", p=128), [], [db_], db_)
                    for tt in range(ntile):
                        a, ab = acc[tt]
                        ps, psb = self.bank()
                        for jj in range(nj):
                            mk.op("pe", lambda e: e.matmul(ps[:, :], hT[:, j0 + jj, tt * 128:(tt + 1) * 128], d_[:, jj, :], start=(jj == 0), stop=(jj == nj - 1)), [hTb, db_], [psb], sig=(jj == nj - 1))
                        mk.op("dve", lambda e: e.tensor_tensor(out=a[:, cs], in0=a[:, cs], in1=ps[:, :], op=ALU.add), [ab, psb], [ab])
            for tt in range(ntile):
                a, ab = acc[tt]
                mk.dma("sp", self.scr[dst][s0 + tt * 128:s0 + (tt + 1) * 128, :], a[:], [ab], [self.scrb[dst]], ab)
        mk.barrier()


@_p(Prog)
def ln_ple(self, tag, src, li, ntok, dst_ap, dstb, p_ap, pre=None):
    mk = self.mk
    Wpg = self.inp["ple_w_gate"][li * D:(li + 1) * D, :]
    Wpp = self.inp["ple_w_proj"][li * 256:(li + 1) * 256, :]
    with ExitStack() as st:
        wres, wresb = pre if pre is not None else self.preload_w(st, tag, Wpg)
        wpp, wppb = self.sb(st, tag + "_wpp", [128, 2, D], BF16)
        for c0 in range(0, D, 1024):
            mk.dma("pool", wpp[:, :, c0:c0 + 1024], Wpp[:, c0:c0 + 1024].rearrange("(kc p) n -> p kc n", p=128), [], [wppb], wppb)
        w_bc, w_bcb, b_bc, b_bcb, stt, stb = self.load_ln(st, li, 1, tag)
        xr = [self.sb(st, tag + "_x%d" % i, [128, D], F32) for i in range(3)]
        xT = [self.sb(st, tag + "_xT%d" % i, [128, KC, 128], BF16) for i in range(3)]
        pin = [self.sb(st, tag + "_p%d" % i, [128, 256], F32) for i in range(3)]
        pT = [self.sb(st, tag + "_pT%d" % i, [128, 2, 128], BF16) for i in range(3)]
        sg = [self.sb(st, tag + "_sg%d" % i, [128, 512], F32) for i in range(2)]
        def loads(i):
            t0 = i * 128
            mk.dma("sp", xr[i % 3][0][:], self.scr[src][t0:t0 + 128, :], [self.scrb[src]], [xr[i % 3][1]], xr[i % 3][1])
            mk.dma("sp", pin[i % 3][0][:], p_ap[t0:t0 + 128, :], [], [pin[i % 3][1]], pin[i % 3][1])
        loads(0)
        for i, t0 in enumerate(range(0, ntok, 128)):
            x, xb = xr[i % 3]
            xTt, xTb = xT[i % 3]
            p_, pb_ = pin[i % 3]
            pTt, pTb = pT[i % 3]
            if t0 + 128 < ntok:
                loads(i + 1)
            if i == 0:
                self.layer_norm(x, xb, stt, stb, w_bc, w_bcb, b_bc, b_bcb)
                self.transpose_to(x, xb, lambda j0, n: xTt[:, j0:j0 + n, :], xTb, KC, False)
                self.transpose_to(p_, pb_, lambda j0, n: pTt[:, j0:j0 + n, :], pTb, 2, False)
            if t0 + 128 < ntok:
                xn, xnb = xr[(i + 1) % 3]
                self.layer_norm(xn, xnb, stt, stb, w_bc, w_bcb, b_bc, b_bcb)
            for cb in range(4):
                cs = slice(cb * 512, (cb + 1) * 512)
                psg, psgb = self.bank()
                psp, pspb = self.bank()
                for kc in range(KC):
                    mk.op("pe", lambda e: e.matmul(psg[:, :], xTt[:, kc, :], wres[:, kc, cs], start=(kc == 0), stop=(kc == KC - 1)), [xTb, wresb], [psgb], sig=(kc == KC - 1))
                for kc in range(2):
                    mk.op("pe", lambda e: e.matmul(psp[:, :], pTt[:, kc, :], wpp[:, kc, cs], start=(kc == 0), stop=(kc == 1)), [pTb, wppb], [pspb], sig=(kc == 1))
                s_, s_b = sg[cb % 2]
                mk.op("act", lambda e: e.activation(out=s_[:], in_=psg[:, :], func=AF.Sigmoid), [psgb], [s_b])
                mk.op("dve", lambda e: e.tensor_tensor(out=s_[:], in0=s_[:], in1=psp[:, :], op=ALU.mult), [s_b, pspb], [s_b])
                mk.op("pool", lambda e: e.tensor_tensor(out=x[:, cs], in0=x[:, cs], in1=s_[:], op=ALU.add), [xb, s_b], [xb])
            mk.dma("sp", dst_ap[t0:t0 + 128, :], x[:], [xb], [dstb], xb)
            if t0 + 128 < ntok:
                xn, xnb = xr[(i + 1) % 3]
                xTn, xTnb = xT[(i + 1) % 3]
                pn, pnb = pin[(i + 1) % 3]
                pTn, pTnb = pT[(i + 1) % 3]
                self.transpose_to(xn, xnb, lambda j0, n: xTn[:, j0:j0 + n, :], xTnb, KC, False)
                self.transpose_to(pn, pnb, lambda j0, n: pTn[:, j0:j0 + n, :], pTnb, 2, False)
        mk.barrier()


@_p(Prog)
def layer0(self):
    self.l0_proj()
    self.l0_gates()
    self.l0_mlstm()
    self.l0_hgrn()
    self.dscr("XA", [F, D], F32)
    self.mix_ln("m0", "Y", self.inp["rec_w_out"], self.inp["x"], None, 0, F, "XA")
    self.dscr("R1", [F, D], F32)
    self.ffn_dense("XA", F, "R1")
    self.dscr("X1", [F, D], F32)
    self.ln_ple("p0", "R1", 0, F, self.scr["X1"], self.scrb["X1"], self.inp["p0"])


def declare_inputs(P, layer1=True):
    P.din("x", [T, D]); P.din("p0", [F, 256]); P.din("consts", [128, 512]); P.din("rmask", [128, T + CH])
    P.din("gbias", [8, 3]); P.din("lbl", [128, 32]); P.din("mlstm_norm_w", [4, 256]); P.din("hgrn_norm_w", [8, 128])
    P.din("ln_w", [4, D]); P.din("ln_b", [4, D]); P.din("w_in", [D, REC_IN]); P.din("rec_w_out", [D, D])
    P.din("ffn_w_gate", [D, FFN]); P.din("ffn_w_up", [D, FFN]); P.din("ffn_w_down", [FFN, D])
    P.din("ple_w_gate", [2 * D, D]); P.din("ple_w_proj", [512, D])
    if layer1:
        P.din("p1", [O, 256]); P.din("na_w_qkv", [D, 3 * D]); P.din("na_w_out", [D, D]); P.din("na_bias", [16 * 3 * 5 * 128, 128])
        P.din("moe_w_router", [D, NE]); P.din("moe_b_router", [1, NE])
        P.din("moe_w_gate", [NE * D, MOE]); P.din("moe_w_up", [NE * D, MOE]); P.din("moe_w_down", [NE * MOE, D])
        P.din("iota", [128, 1024])


def core_inputs(inp, core, layer1=True, shared=None):
    b, half = core // 2, core % 2
    flip = half == 1
    sh = shared if shared is not None else {}

    def cached(key, fn):
        if key not in sh:
            sh[key] = fn()
        return sh[key]
    f32 = lambda a: np.ascontiguousarray(a, dtype=np.float32)
    x = inp["x"][b]
    p = inp["p"][:, b]
    if flip:
        x = x[::-1]
        p = p[:, ::-1]
    o = {
        "x": f32(x), "p0": f32(p[0, :F]),
        "consts": cached("consts", make_consts), "rmask": cached("rmask", make_rmask),
        "gbias": make_gbias(inp["mlstm_gate_bias"][0], flip), "lbl": make_lbl(inp["hgrn_lb_logits"], flip),
        "mlstm_norm_w": f32(inp["mlstm_norm_w"][0].reshape(4, 256)), "hgrn_norm_w": f32(inp["hgrn_norm_w"][0].reshape(8, 128)),
        "ln_w": f32(inp["ln_w"].reshape(4, D)), "ln_b": f32(inp["ln_b"].reshape(4, D)),
        "w_in": cached(("w_in", flip), lambda: f32(inp["rec_w_in"][0][:, win_perm(flip)])),
        "rec_w_out": f32(inp["rec_w_out"][0]),
        "ffn_w_gate": f32(inp["ffn_w_gate"][0]), "ffn_w_up": f32(inp["ffn_w_up"][0]), "ffn_w_down": f32(inp["ffn_w_down"][0]),
        "ple_w_gate": f32(inp["ple_w_gate"].reshape(2 * D, D)), "ple_w_proj": f32(inp["ple_w_proj"].reshape(512, D)),
    }
    if layer1:
        o.update({
            "p1": f32(p[1, :O]), "na_w_qkv": f32(inp["na_w_qkv"][0]), "na_w_out": f32(inp["na_w_out"][0]),
            "na_bias": cached(("na_bias", flip), lambda: make_na_bias(inp["na_rpb"][0], flip)),
            "moe_w_router": f32(inp["moe_w_router"][0]), "moe_b_router": f32(inp["moe_b_router"][0].reshape(1, NE)),
            "moe_w_gate": f32(inp["moe_w_gate"][0].reshape(NE * D, MOE)), "moe_w_up": f32(inp["moe_w_up"][0].reshape(NE * D, MOE)),
            "moe_w_down": f32(inp["moe_w_down"][0].reshape(NE * MOE, D)),
            "iota": cached("iota", lambda: np.tile(np.arange(1024, dtype=np.float32)[None, :], (128, 1))),
        })
    return o


def make_na_bias(rpb, flip):
    out = np.full((16, 3, 5, 128, 128), -30000.0, np.float32)
    G = (lambda a: 63 - a) if flip else (lambda a: a)
    for k, p in ((0, 0), (1, 1), (2, 2)):
        ws = 0 if p < 2 else 2 * p - 4
        for c in range(5):
            ki = np.arange(128)
            kr = G(ws + 2 * c + ki // 64)[:, None]
            kc = G(ki % 64)[:, None]
            r = G(2 * p + ki // 64)[None, :]
            qc = G(ki % 64)[None, :]
            rs = np.clip(r - 4, 0, 56)
            vr = (kr >= rs) & (kr < rs + 8)
            cs = np.clip(qc - 8, 0, 48)
            vc = (kc >= cs) & (kc < cs + 16)
            dr = np.clip(kr - r + 7, 0, 14)
            dc = np.clip(kc - qc + 15, 0, 30)
            vals = rpb[:, dr, dc]
            out[:, k, c] = np.where((vr & vc)[None], vals, np.float32(-30000.0))
    return np.ascontiguousarray(out.reshape(16 * 3 * 5 * 128, 128))


@_p(Prog)
def l1_qkv(self):
    mk = self.mk
    S = self.dscr
    S("QT", [D, O], BF16); S("KT", [D, F], BF16); S("V1", [F, D], BF16)
    G = lambda c0, w, o, k, dst, dc0=0, scale=None: dict(c0=c0, w=w, orient=o, kind=k, dst=dst, dc0=dc0, scale=scale)
    with ExitStack() as st:
        xT, xTb = self.sb(st, "q_xT", [128, KC, F], BF16)
        xin = [self.sb(st, "q_xin%d" % i, [128, D], F32) for i in range(2)]
        wsl = [self.sb(st, "q_wp%d" % i, [128, KC, 512], BF16) for i in range(2)]
        stg = {False: [self.sb(st, "q_sg32_%d" % i, [128, 512], F32) for i in range(4)],
               True: [self.sb(st, "q_sg16_%d" % i, [128, 512], BF16) for i in range(4)]}
        self.build_xT(st, self.scr["X1"], self.scrb["X1"], xT, xTb, 0, F, xin)
        self.proj(st, xT, xTb, O, self.inp["na_w_qkv"], [G(0, D, "b", "scale", "QT", scale=128 ** -0.5)], 0, wsl, stg)
        self.proj(st, xT, xTb, F, self.inp["na_w_qkv"], [G(D, D, "b", "copy", "KT"), G(2 * D, D, "a", "copy", "V1")], 0, wsl, stg)
        mk.barrier()


@_p(Prog)
def l1_attn(self):
    mk = self.mk
    self.dscr("AO", [O, D], BF16)
    NP = O // 128
    with ExitStack() as st:
        KTs = [self.sb(st, "a_KT%d" % i, [128, F], BF16) for i in range(2)]
        QTs = [self.sb(st, "a_QT%d" % i, [128, O], BF16) for i in range(2)]
        Vs = [self.sb(st, "a_V%d" % i, [128, F // 128, 129], BF16) for i in range(2)]
        biass = [self.sb(st, "a_bias%d" % i, [128, 15, 128], F32) for i in range(2)]
        AOs = [self.sb(st, "a_AO%d" % i, [128, NP, 128], BF16) for i in range(2)]

        def loads(h):
            rows = slice(h * 128, (h + 1) * 128)
            KTh, KThb = KTs[h % 2]; QTh, QThb = QTs[h % 2]; V, Vb = Vs[h % 2]; bias, biasb = biass[h % 2]
            mk.dma("sp", KTh[:], self.scr["KT"][rows, :], [self.scrb["KT"]], [KThb], KThb)
            mk.dma("sp", QTh[:], self.scr["QT"][rows, :], [self.scrb["QT"]], [QThb], QThb)
            mk.dma("sp", V[:, :, 0:128], self.scr["V1"][:, rows].rearrange("(c p) e -> p c e", p=128), [self.scrb["V1"]], [Vb], Vb)
            mk.op("pool", lambda e: e.memset(V[:, :, 128:129], 1.0), [], [Vb])
            mk.dma("sp", bias[:], self.inp["na_bias"][h * 15 * 128:(h + 1) * 15 * 128, :].rearrange("(k p) q -> p k q", p=128), [], [biasb], biasb)
        loads(0)
        ssb = [self.sb(st, "a_s%d" % i, [128, 640], F32) for i in range(2)]
        pT = [self.sb(st, "a_pT%d" % i, [128, 640], BF16) for i in range(2)]
        rc = [self.sb(st, "a_rc%d" % i, [128, 1], F32) for i in range(4)]
        for h in range(16):
            rows = slice(h * 128, (h + 1) * 128)
            KTh, KThb = KTs[h % 2]; QTh, QThb = QTs[h % 2]; V, Vb = Vs[h % 2]; bias, biasb = biass[h % 2]
            AOh, AOhb = AOs[h % 2]
            if h + 1 < 16:
                loads(h + 1)
            def scores(p):
                ws = 0 if p < 2 else 2 * p - 4
                qs = slice(p * 128, (p + 1) * 128)
                for c in range(5):
                    kt0 = (ws // 2 + c) * 128
                    mk.op("pe", lambda e: e.matmul(self.psw[:, c * 128:(c + 1) * 128], KTh[:, kt0:kt0 + 128], QTh[:, qs], start=True, stop=True),
                          [KThb, QThb], [self.psb[4], self.psb[5]], sig=(c == 4))
            scores(0)
            for p in range(NP):
                k = p if p < 2 else 2
                ws = 0 if p < 2 else 2 * p - 4
                s_, s_b = ssb[p % 2]
                mk.op("dve", lambda e: e.tensor_tensor(out=s_[:].rearrange("p (c q) -> p c q", c=5), in0=self.psw[:, 0:640].rearrange("p (c q) -> p c q", c=5), in1=bias[:, k * 5:(k + 1) * 5, :], op=ALU.add),
                      [self.psb[4], self.psb[5], biasb], [s_b])
                pt, ptb = pT[p % 2]
                mk.op("act", lambda e: e.activation(out=pt[:], in_=s_[:], func=AF.Exp), [s_b], [ptb])
                if p + 1 < NP:
                    scores(p + 1)
                po, pob = self.bank()
                for c in range(5):
                    mk.op("pe", lambda e: e.matmul(po[:, 0:129], pt[:, c * 128:(c + 1) * 128], V[:, ws // 2 + c, :], start=(c == 0), stop=(c == 4)), [ptb, Vb], [pob], sig=(c == 4))
                r_, r_b = rc[p % 4]
                mk.op("dve", lambda e: e.reciprocal(out=r_[:], in_=po[:, 128:129]), [pob], [r_b])
                mk.op("act", lambda e: e.activation(out=AOh[:, p, :], in_=po[:, 0:128], func=AF.Copy, scale=r_[:, 0:1]), [pob, r_b], [AOhb])
            mk.dma("sp", self.scr["AO"][:, rows].rearrange("(c p) e -> p c e", p=128), AOh[:], [AOhb], [self.scrb["AO"]], AOhb)
        mk.barrier()


def _idma(self, out, in_, idx_ap, reads, writes, owner):
    need = self._deps(reads, writes)
    fresh = self._acquire(owner)
    if owner.dcnt and not fresh:
        k = ("d", owner)
        if need.get(k, 0) < owner.dcnt * 16:
            need[k] = owner.dcnt * 16
    self._wait("pool", need)
    ins = self.nc.gpsimd.indirect_dma_start(out=out, out_offset=None, in_=in_, in_offset=bass.IndirectOffsetOnAxis(ap=idx_ap, axis=0))
    owner.dcnt += 1
    ins.then_inc(owner.dsem, 16)
    self._record(("d", owner), owner.dcnt * 16, reads, writes)


MK.idma = _idma
NT_O = O // 128


@_p(Prog)
def l1_route(self):
    mk = self.mk
    S = self.dscr
    S("XG", [NE * D, CAP], BF16); S("IDXd", [128, 2 * NT_O], U32); S("Gd", [128, 2 * NT_O], F32)
    with ExitStack() as st:
        xcb, xcbb = self.sb(st, "r_xcb", [128, NT_O, D], BF16)
        wr, wrb = self.sb(st, "r_wr", [128, KC, NE], F32)
        br, brb = self.sb(st, "r_br", [128, NE], F32)
        ecap, ecapb = self.sb(st, "r_ecap", [128, NE], F32)
        iot, iotb = self.sb(st, "r_iot", [128, CAP], F32)
        tris, trisb = self.sb(st, "r_tris", [128, 128], BF16)
        ones, onesb = self.sb(st, "r_ones", [128, 128], BF16)
        ctmp, ctmpb = self.sb(st, "r_ctmp", [128, 256], F32)
        Mf, Mfb = self.sb(st, "r_Mf", [128, NT_O, NE], F32)
        Mbf, Mbfb = self.sb(st, "r_Mbf", [128, NT_O, NE], BF16)
        m1h, m1hb = self.sb(st, "r_m1h", [128, NT_O, NE], F32)
        m2h, m2hb = self.sb(st, "r_m2h", [128, NT_O, NE], F32)
        posf, posfb = self.sb(st, "r_pos", [128, NT_O, NE], F32)
        Gt, Gtb = self.sb(st, "r_G", [128, 2 * NT_O], F32)
        idxf, idxfb = self.sb(st, "r_idxf", [128, 2 * NT_O], F32)
        idxu, idxub = self.sb(st, "r_idxu", [128, 2 * NT_O], U32)
        xin = [self.sb(st, "r_x%d" % i, [128, D], F32) for i in range(2)]
        xT32 = [self.sb(st, "r_xT%d" % i, [128, KC, 128], F32) for i in range(2)]
        lg = [self.sb(st, "r_lg%d" % i, [128, 24], F32) for i in range(2)]
        Sels = [self.sb(st, "r_Sel%d" % i, [128, NT_O, CAP], BF16) for i in range(2)]
        xgs = [self.sb(st, "r_xg%d" % i, [128, CAP], BF16) for i in range(2)]
        mk.dma("sp", wr[:], self.inp["moe_w_router"].rearrange("(kc p) e -> p kc e", p=128), [], [wrb], wrb)
        mk.dma("sp", br[:], self.inp["moe_b_router"].to_broadcast([128, NE]), [], [brb], brb)
        mk.dma("sp", iot[:], self.inp["iota"][:, 0:CAP], [], [iotb], iotb)
        mk.dma("sp", ctmp[:], self.inp["consts"][:, 256:512], [], [ctmpb], ctmpb)
        mk.op("dve", lambda e: e.tensor_copy(out=tris[:], in_=ctmp[:, 0:128]), [ctmpb], [trisb])
        mk.op("dve", lambda e: e.tensor_copy(out=ones[:], in_=ctmp[:, 128:256]), [ctmpb], [onesb])
        mk.op("dve", lambda e: e.tensor_scalar(out=ecap[:], in0=iot[:, 0:NE], scalar1=float(CAP), scalar2=None, op0=ALU.mult), [iotb], [ecapb])
        for i in range(NT_O):
            x, xb = xin[i % 2]
            xT, xTb = xT32[i % 2]
            l_, l_b = lg[i % 2]
            mk.dma("sp", x[:], self.scr["XC"][i * 128:(i + 1) * 128, :], [self.scrb["XC"]], [xb], xb)
            mk.op("act", lambda e: e.copy(out=xcb[:, i, :], in_=x[:]), [xb], [xcbb])
            self.transpose_to(x, xb, lambda j0, n: xT[:, j0:j0 + n, :], xTb, KC, False)
            ps, psb = self.bank()
            for kc in range(KC):
                mk.op("pe", lambda e: e.matmul(ps[:, 0:NE], xT[:, kc, :], wr[:, kc, :], start=(kc == 0), stop=(kc == KC - 1)), [xTb, wrb], [psb], sig=(kc == KC - 1))
            mk.op("dve", lambda e: e.tensor_tensor(out=l_[:, 0:8], in0=ps[:, 0:NE], in1=br[:], op=ALU.add), [psb, brb], [l_b])
            mk.op("dve", lambda e: e.max(out=l_[:, 8:16], in_=l_[:, 0:8]), [l_b], [l_b])
            mk.op("dve", lambda e: e.tensor_scalar(out=Mf[:, i, :], in0=l_[:, 0:8], scalar1=l_[:, 9:10], scalar2=None, op0=ALU.is_ge), [l_b], [Mfb])
            mk.op("dve", lambda e: e.tensor_scalar(out=m1h[:, i, :], in0=l_[:, 0:8], scalar1=l_[:, 8:9], scalar2=None, op0=ALU.is_ge), [l_b], [m1hb])
            mk.op("dve", lambda e: e.tensor_tensor(out=m2h[:, i, :], in0=Mf[:, i, :], in1=m1h[:, i, :], op=ALU.subtract), [Mfb, m1hb], [m2hb])
            mk.op("dve", lambda e: e.tensor_tensor(out=l_[:, 16:17], in0=l_[:, 8:9], in1=l_[:, 9:10], op=ALU.subtract), [l_b], [l_b])
            mk.op("act", lambda e: e.activation(out=Gt[:, 2 * i:2 * i + 1], in_=l_[:, 16:17], func=AF.Sigmoid), [l_b], [Gtb])
            mk.op("dve", lambda e: e.tensor_scalar(out=Gt[:, 2 * i + 1:2 * i + 2], in0=Gt[:, 2 * i:2 * i + 1], scalar1=-1.0, scalar2=1.0, op0=ALU.mult, op1=ALU.add), [Gtb], [Gtb])
        mk.op("dve", lambda e: e.tensor_copy(out=Mbf[:], in_=Mf[:]), [Mfb], [Mbfb])
        for i in range(NT_O):
            ps, psb = self.bank()
            mk.op("pe", lambda e: e.matmul(ps[:, 0:NE], tris[:], Mbf[:, i, :], start=True, stop=(i == 0)), [trisb, Mbfb], [psb], sig=(i == 0))
            for i2 in range(i):
                mk.op("pe", lambda e: e.matmul(ps[:, 0:NE], ones[:], Mbf[:, i2, :], start=False, stop=(i2 == i - 1)), [onesb, Mbfb], [psb], sig=(i2 == i - 1))
            mk.op("act", lambda e: e.copy(out=posf[:, i, :], in_=ps[:, 0:NE]), [psb], [posfb])
            l_, l_b = lg[i % 2]
            for k, mh, mhb in ((0, m1h, m1hb), (1, m2h, m2hb)):
                mk.op("dve", lambda e: e.tensor_tensor(out=l_[:, 0:8], in0=posf[:, i, :], in1=ecap[:], op=ALU.add), [posfb, ecapb], [l_b])
                mk.op("dve", lambda e: e.tensor_tensor(out=l_[:, 0:8], in0=l_[:, 0:8], in1=mh[:, i, :], op=ALU.mult), [l_b, mhb], [l_b])
                mk.op("dve", lambda e: e.tensor_reduce(out=idxf[:, 2 * i + k:2 * i + k + 1], in_=l_[:, 0:8], axis=AX.X, op=ALU.add), [l_b], [idxfb])
        mk.op("dve", lambda e: e.tensor_copy(out=idxu[:], in_=idxf[:]), [idxfb], [idxub])
        mk.dma("sp", self.scr["IDXd"], idxu[:], [idxub], [self.scrb["IDXd"]], idxub)
        mk.dma("sp", self.scr["Gd"], Gt[:], [Gtb], [self.scrb["Gd"]], Gtb)
        for ex in range(NE):
            Sel, Selb = Sels[ex % 2]
            for i in range(NT_O):
                eng = "dve" if i % 4 else "pool"
                mk.op(eng, lambda e: e.tensor_scalar(out=Sel[:, i, :], in0=iot[:], scalar1=posf[:, i, ex:ex + 1], scalar2=Mf[:, i, ex:ex + 1], op0=ALU.is_equal, op1=ALU.mult),
                      [iotb, posfb, Mfb], [Selb])
            for kc in range(KC):
                for i in range(NT_O):
                    mk.op("pe", lambda e: e.matmul(self.psw[:, 0:512], xcb[:, i, kc * 128:(kc + 1) * 128], Sel[:, i, 0:512], start=(i == 0), stop=(i == NT_O - 1)), [xcbb, Selb], [self.psb[4]], sig=False)
                    mk.op("pe", lambda e: e.matmul(self.psw[:, 512:CAP], xcb[:, i, kc * 128:(kc + 1) * 128], Sel[:, i, 512:CAP], start=(i == 0), stop=(i == NT_O - 1)), [xcbb, Selb], [self.psb[5]], sig=(i == NT_O - 1))
                xg, xgb = xgs[kc % 2]
                if kc % 2:
                    mk.op("act", lambda e: e.copy(out=xg[:], in_=self.psw[:, 0:CAP]), [self.psb[4], self.psb[5]], [xgb])
                else:
                    mk.op("dve", lambda e: e.tensor_copy(out=xg[:], in_=self.psw[:, 0:CAP]), [self.psb[4], self.psb[5]], [xgb])
                r0 = ex * D + kc * 128
                mk.dma("sp", self.scr["XG"][r0:r0 + 128, :], xg[:], [xgb], [self.scrb["XG"]], xgb)
        mk.barrier()


@_p(Prog)
def l1_experts(self):
    mk = self.mk
    self.dscr("YALL", [NE * CAP, D], F32)
    NH = MOE // 128
    NS = CAP // 128
    HB = CAP // 2
    Wg, Wu, Wd = self.inp["moe_w_gate"], self.inp["moe_w_up"], self.inp["moe_w_down"]
    with ExitStack() as st:
        xgT, xgTb = self.sb(st, "e_xgT", [128, KC, CAP], BF16)
        hT, hTb = self.sb(st, "e_hT", [128, NH, CAP], BF16)
        acc = [self.sb(st, "e_acc%d" % i, [128, D], F32) for i in range(NS)]
        wg = [self.sb(st, "e_wg%d" % i, [128, KC, 128], BF16) for i in range(2)]
        wu = [self.sb(st, "e_wu%d" % i, [128, KC, 128], BF16) for i in range(2)]
        wd = [self.sb(st, "e_wd%d" % i, [128, 8, 512], BF16) for i in range(2)]
        sg = [self.sb(st, "e_sg%d" % i, [128, HB], F32) for i in range(2)]
        for ex in range(NE):
            mk.dma("sp", xgT[:], self.scr["XG"][ex * D:(ex + 1) * D, :].rearrange("(kc p) s -> p kc s", p=128), [self.scrb["XG"]], [xgTb], xgTb)
            for j in range(NH):
                g_, gb_ = wg[j % 2]
                u_, ub_ = wu[j % 2]
                self.load_w_panel(g_, gb_, Wg[ex * D:(ex + 1) * D, :], j * 128, 128)
                self.load_w_panel(u_, ub_, Wu[ex * D:(ex + 1) * D, :], j * 128, 128)
                for blk in range(2):
                    bs = slice(blk * HB, (blk + 1) * HB)
                    psg, psgb = self.bank()
                    psu, psub = self.bank()
                    for kc in range(KC):
                        mk.op("pe", lambda e: e.matmul(psg[:, 0:HB], g_[:, kc, :], xgT[:, kc, bs], start=(kc == 0), stop=(kc == KC - 1)), [gb_, xgTb], [psgb], sig=(kc == KC - 1))
                    for kc in range(KC):
                        mk.op("pe", lambda e: e.matmul(psu[:, 0:HB], u_[:, kc, :], xgT[:, kc, bs], start=(kc == 0), stop=(kc == KC - 1)), [ub_, xgTb], [psub], sig=(kc == KC - 1))
                    s_, s_b = sg[blk]
                    mk.op("act", lambda e: e.activation(out=s_[:], in_=psg[:, 0:HB], func=AF.Silu), [psgb], [s_b])
                    mk.op("dve", lambda e: e.tensor_tensor(out=hT[:, j, bs], in0=s_[:], in1=psu[:, 0:HB], op=ALU.mult), [s_b, psub], [hTb])
            pi = 0
            for cb in range(4):
                cs = slice(cb * 512, (cb + 1) * 512)
                for j0 in range(0, NH, 8):
                    d_, db_ = wd[pi % 2]
                    pi += 1
                    r0 = ex * MOE + j0 * 128
                    mk.dma("pool", d_[:], Wd[r0:r0 + 1024, cs].rearrange("(j p) n -> p j n", p=128), [], [db_], db_)
                    for tt in range(NS):
                        a, ab = acc[tt]
                        ps, psb = self.bank()
                        for jj in range(8):
                            mk.op("pe", lambda e: e.matmul(ps[:, :], hT[:, j0 + jj, tt * 128:(tt + 1) * 128], d_[:, jj, :], start=(jj == 0), stop=(jj == 7)), [hTb, db_], [psb], sig=(jj == 7))
                        if j0 == 0:
                            mk.op("act", lambda e: e.copy(out=a[:, cs], in_=ps[:, :]), [psb], [ab])
                        else:
                            mk.op("dve", lambda e: e.tensor_tensor(out=a[:, cs], in0=a[:, cs], in1=ps[:, :], op=ALU.add), [ab, psb], [ab])
            for tt in range(NS):
                a, ab = acc[tt]
                r0 = ex * CAP + tt * 128
                mk.dma("sp", self.scr["YALL"][r0:r0 + 128, :], a[:], [ab], [self.scrb["YALL"]], ab)
        mk.barrier()


@_p(Prog)
def l1_combine(self):
    mk = self.mk
    self.dscr("R2", [O, D], F32)
    with ExitStack() as st:
        idx, idxb = self.sb(st, "c_idx", [128, 2 * NT_O], U32)
        g, gb = self.sb(st, "c_g", [128, 2 * NT_O], F32)
        xr = [self.sb(st, "c_x%d" % i, [128, D], F32) for i in range(3)]
        y1 = [self.sb(st, "c_y1%d" % i, [128, D], F32) for i in range(3)]
        y2 = [self.sb(st, "c_y2%d" % i, [128, D], F32) for i in range(3)]
        mk.dma("sp", idx[:], self.scr["IDXd"], [self.scrb["IDXd"]], [idxb], idxb)
        mk.dma("sp", g[:], self.scr["Gd"], [self.scrb["Gd"]], [gb], gb)
        def loads(i):
            mk.dma("sp", xr[i % 3][0][:], self.scr["XC"][i * 128:(i + 1) * 128, :], [self.scrb["XC"]], [xr[i % 3][1]], xr[i % 3][1])
            mk.idma(y1[i % 3][0][:], self.scr["YALL"], idx[:, 2 * i:2 * i + 1], [self.scrb["YALL"], idxb], [y1[i % 3][1]], y1[i % 3][1])
            mk.idma(y2[i % 3][0][:], self.scr["YALL"], idx[:, 2 * i + 1:2 * i + 2], [self.scrb["YALL"], idxb], [y2[i % 3][1]], y2[i % 3][1])
        loads(0)
        for i in range(NT_O):
            x, xb = xr[i % 3]
            a, ab = y1[i % 3]
            b, bb = y2[i % 3]
            if i + 1 < NT_O:
                loads(i + 1)
            mk.op("act", lambda e: e.mul(x[:], x[:], ALPHA), [xb], [xb])
            mk.op("dve", lambda e: e.scalar_tensor_tensor(out=x[:], in0=a[:], scalar=g[:, 2 * i:2 * i + 1], in1=x[:], op0=ALU.mult, op1=ALU.add), [ab, gb, xb], [xb])
            mk.op("dve", lambda e: e.scalar_tensor_tensor(out=x[:], in0=b[:], scalar=g[:, 2 * i + 1:2 * i + 2], in1=x[:], op0=ALU.mult, op1=ALU.add), [bb, gb, xb], [xb])
            mk.dma("sp", self.scr["R2"][i * 128:(i + 1) * 128, :], x[:], [xb], [self.scrb["R2"]], xb)
        mk.barrier()


@_p(Prog)
def layer1(self, out_ap, outb):
    self.l1_qkv()
    with ExitStack() as st:
        pre = self.preload_w(st, "m1", self.inp["na_w_out"])
        self.l1_attn()
        self.dscr("XC", [O, D], F32)
        self.mix_ln("m1", "AO", self.inp["na_w_out"], self.scr["X1"], self.scrb["X1"], 1, O, "XC", pre=pre)
    self.l1_route()
    self.l1_experts()
    with ExitStack() as st:
        pre = self.preload_w(st, "p1", self.inp["ple_w_gate"][D:2 * D, :])
        self.l1_combine()
        self.ln_ple("p1", "R2", 1, O, out_ap, outb, self.inp["p1"], pre=pre)


def build_full():
    nc = bass.Bass("TRN2", target_bir_lowering=False)
    with ExitStack() as es:
        P = Prog(nc, es)
        declare_inputs(P, layer1=True)
        out = nc.dram_tensor("out", [O, D], F32, kind="ExternalOutput").ap()
        outb = P.mk.buf("out")
        P.outs.append(outb)
        with ExitStack() as st:
            P.consts(st)
            P.small_consts(st)
            P.layer0()
            P.layer1(out, outb)
            P.mk.finish(P.outs)
    return nc


def kernel(**inputs):
    inp = {k: np.asarray(v) for k, v in inputs.items()}
    nc = build_full()
    shared = {}
    in_maps = [core_inputs(inp, c, layer1=True, shared=shared) for c in range(8)]
    for k in list(in_maps[0].keys()):
        for c in range(1, 8):
            a, b = in_maps[0][k], in_maps[c][k]
            if a is not b and a.shape == b.shape and k not in ("x", "p0", "p1", "gbias", "lbl", "w_in", "na_bias"):
                in_maps[c][k] = a
    res = run_bass_kernel_spmd(nc, in_maps, core_ids=list(range(8)))
    out = np.zeros((4, T, D), np.float32)
    for c in range(8):
        b, half = c // 2, c % 2
        oc = np.asarray(res.results[c]["out"], np.float32)
        if half == 0:
            out[b, 0:O] = oc
        else:
            out[b, O:T] = oc[::-1]
    return out
```

```python
import numpy as np
import concourse.bass as bass
import concourse.mybir as mybir
from concourse.bass_utils import run_bass_kernel_spmd
from contextlib import ExitStack

F32 = mybir.dt.float32
BF16 = mybir.dt.bfloat16
U32 = mybir.dt.uint32
AF = mybir.ActivationFunctionType
ALU = mybir.AluOpType
AX = mybir.AxisListType

D = 2048
T = 4096
F = 2304
O = 2048
KC = D // 128
CH = 64
NCF = F // CH
NCT = T // CH
REC_IN = 8208
FFN = 5632
MOE = 7168
NE = 8
SK_O, SK_E = 1, 2
SK_P = 2
NBK = 4
CAP = 640
ALPHA = 4 ** 0.25
LN_EPS = 1e-5
RMS_EPS = 1e-6


class Buf:
    __slots__ = ("name", "w", "readers", "dsem", "dcnt", "rel")

    def __init__(self, name):
        self.name = name
        self.w = {}
        self.readers = {}
        self.dsem = None
        self.dcnt = 0
        self.rel = False


class MK:
    def __init__(self, nc, es):
        self.nc = nc
        self.es = es
        self.eng = dict(pe=nc.tensor, act=nc.scalar, dve=nc.vector, pool=nc.gpsimd, sp=nc.sync)
        self.sem = {k: es.enter_context(nc.semaphore("sem_" + k)) for k in self.eng}
        self.cnt = {k: 0 for k in self.eng}
        self.seen = {k: {} for k in self.eng}
        self.owners = []
        self.free_sems = []
        self.nsem = 0
        self.nb = 0

    def buf(self, name=None):
        self.nb += 1
        return Buf(name or ("b%d" % self.nb))

    def _acquire(self, owner):
        if owner.dsem is None or owner.rel:
            if self.free_sems:
                owner.dsem, owner.dcnt = self.free_sems.pop()
            else:
                owner.dsem = self.es.enter_context(self.nc.semaphore("ds%d" % self.nsem))
                self.nsem += 1
                owner.dcnt = 0
            owner.rel = False
            self.owners.append(owner)
            return True
        return False

    def _deps(self, reads, writes):
        need = {}
        for b in reads:
            for k, v in b.w.items():
                if need.get(k, 0) < v:
                    need[k] = v
        for b in writes:
            for k, v in b.w.items():
                if need.get(k, 0) < v:
                    need[k] = v
            for k, v in b.readers.items():
                if need.get(k, 0) < v:
                    need[k] = v
        return need

    def _wait(self, e, need):
        for key, val in need.items():
            if key[0] == "e" and key[1] == "pe" and e == "pe":
                continue
            if self.seen[e].get(key, 0) >= val:
                continue
            sem = self.sem[key[1]] if key[0] == "e" else key[1].dsem
            self.eng[e].wait_ge(sem, val)
            self.seen[e][key] = val

    def _record(self, key, val, reads, writes):
        for b in reads:
            if b.readers.get(key, 0) < val:
                b.readers[key] = val
        for b in writes:
            b.w[key] = val
            b.readers = {}

    def op(self, e, fn, reads=(), writes=(), sig=True):
        self._wait(e, self._deps(reads, writes))
        ins = fn(self.eng[e])
        if sig:
            self.cnt[e] += 1
            ins.then_inc(self.sem[e], 1)
            t = self.cnt[e]
        else:
            t = self.cnt[e] + 1
        self._record(("e", e), t, reads, writes)
        return ins

    def dma(self, q, out, in_, reads, writes, owner, **kw):
        need = self._deps(reads, writes)
        fresh = self._acquire(owner)
        if owner.dcnt and not fresh:
            k = ("d", owner)
            if need.get(k, 0) < owner.dcnt * 16:
                need[k] = owner.dcnt * 16
        self._wait(q, need)
        ins = self.eng[q].dma_start(out=out, in_=in_, **kw)
        owner.dcnt += 1
        ins.then_inc(owner.dsem, 16)
        self._record(("d", owner), owner.dcnt * 16, reads, writes)
        return ins

    def barrier(self):
        need = {}
        for e in self.eng:
            if e != "sp" and self.cnt[e]:
                need[("e", e)] = self.cnt[e]
        for o in self.owners:
            if o.dcnt:
                need[("d", o)] = o.dcnt * 16
        self._wait("sp", need)
        self.nc.sync.sem_inc(self.sem["sp"], 1)
        self.cnt["sp"] += 1
        for e in self.eng:
            if e != "sp":
                self.eng[e].wait_ge(self.sem["sp"], self.cnt["sp"])
                self.seen[e][("e", "sp")] = self.cnt["sp"]
        for o in self.owners:
            o.rel = True
            self.free_sems.append((o.dsem, o.dcnt))
        self.owners = []

    def finish(self, bufs):
        need = {}
        for b in bufs:
            for k, v in b.w.items():
                need[k] = max(need.get(k, 0), v)
        self._wait("sp", need)


C_QA, C_KA, C_VA, C_OA, C_GI, C_GF, C_QB, C_FF, C_FB, C_IB, C_GB = (
    0, 512, 1024, 2048, 3072, 3080, 3088, 4112, 5136, 6160, 7184)


class Prog:
    def __init__(self, nc, es, dbg=(), stop=None):
        self.nc = nc
        self.es = es
        self.mk = MK(nc, es)
        self.dbg = set(dbg)
        self.stop = stop
        self.outs = []
        self.inp = {}
        self.scr = {}
        self.scrb = {}
        mk = self.mk
        self.ps = [es.enter_context(nc.psum_tensor("ps%d" % i, [128, 512], F32)) for i in range(4)]
        self.psw = es.enter_context(nc.psum_tensor("psw", [128, 1024], F32))
        self.ps += [self.psw[:, 0:512], self.psw[:, 512:1024]]
        self.psb = [mk.buf("ps%d" % i) for i in range(6)]
        self.pst2 = es.enter_context(nc.psum_tensor("pst2", [128, 1024], BF16))
        self.pst2b = mk.buf("pst2")
        self.pst = es.enter_context(nc.psum_tensor("pst", [128, 1024], BF16))
        self.pstb = mk.buf("pst")
        self.pstsb = [mk.buf("psts%d" % i) for i in range(8)]
        self.pstall = self.pstsb
        self.rr = 0

    def din(self, name, shape, dt=F32):
        self.inp[name] = self.nc.dram_tensor(name, list(shape), dt, kind="ExternalInput").ap()
        return self.inp[name]

    def dscr(self, name, shape, dt):
        kind = "ExternalOutput" if name in self.dbg else "Internal"
        self.scr[name] = self.nc.dram_tensor(name, list(shape), dt, kind=kind).ap()
        self.scrb[name] = self.mk.buf(name)
        if name in self.dbg:
            self.outs.append(self.scrb[name])
        return self.scr[name]

    def sb(self, stack, name, shape, dt):
        t = stack.enter_context(self.nc.sbuf_tensor(name, list(shape), dt))
        return t, self.mk.buf(name)

    def bank(self, n=4):
        i = self.rr % n
        self.rr += 1
        return self.ps[i], self.psb[i]

    def consts(self, st):
        mk, nc = self.mk, self.nc
        self.ident, self.identb_ = self.sb(st, "ident", [128, 128], F32)
        self.identh, self.identhb = self.sb(st, "identh", [128, 128], BF16)
        self.mut, self.mutb = self.sb(st, "mut", [64, 64], F32)
        self.mlt, self.mltb = self.sb(st, "mlt", [64, 64], F32)
        c = self.inp["consts"]
        mk.dma("sp", self.ident[:], c[:, 0:128], [], [self.identb_], self.identb_)
        mk.dma("sp", self.mut[:], c[0:64, 128:192], [], [self.mutb], self.mutb)
        mk.dma("sp", self.mlt[:], c[0:64, 192:256], [], [self.mltb], self.mltb)
        mk.op("dve", lambda e: e.tensor_copy(out=self.identh[:], in_=self.ident[:]), [self.identb_], [self.identhb])

    def transpose_to(self, src, srcb, dst_fn, dstb, nblk, bf):
        mk = self.mk
        if bf:
            per = 8
            for j0 in range(0, nblk, per):
                n = min(per, nblk - j0)
                for j in range(n):
                    mk.op("pe", lambda e, j=j: e.transpose(self.pst[:, j * 128:(j + 1) * 128], src[:, (j0 + j) * 128:(j0 + j + 1) * 128], self.identh[:]),
                          [srcb, self.identhb], self.pstsb, sig=(j == n - 1))
                mk.op("act", lambda e: e.copy(out=dst_fn(j0, n), in_=self.pst[:, 0:n * 128].rearrange("p (j t) -> p j t", t=128)),
                      self.pstsb, [dstb])
        else:
            per = 4
            k = 0
            for j0 in range(0, nblk, per):
                n = min(per, nblk - j0)
                pi = 4 + (k % 2)
                k += 1
                ps, psb = self.ps[pi], self.psb[pi]
                for j in range(n):
                    mk.op("pe", lambda e, j=j: e.transpose(ps[:, j * 128:(j + 1) * 128], src[:, (j0 + j) * 128:(j0 + j + 1) * 128], self.ident[:]),
                          [srcb, self.identb_], [psb], sig=(j == n - 1))
                eng = "act" if (k % 2) else "dve"
                if eng == "act":
                    mk.op("act", lambda e: e.copy(out=dst_fn(j0, n), in_=ps[:, 0:n * 128].rearrange("p (j t) -> p j t", t=128)), [psb], [dstb])
                else:
                    mk.op("dve", lambda e: e.tensor_copy(out=dst_fn(j0, n), in_=ps[:, 0:n * 128].rearrange("p (j t) -> p j t", t=128)), [psb], [dstb])

    def load_w_panel(self, wt, wb, W, c0, w, kc=KC):
        self.mk.dma("pool", wt[:, 0:kc, 0:w], W[:, c0:c0 + w].rearrange("(kc p) n -> p kc n", p=128), [], [wb], wb)

    def layer_norm(self, x, xb, st_t, st_b, w_bc, w_bcb, b_bc, b_bcb, n=128):
        mk = self.mk
        stats, mv, sd = st_t
        for j in range(4):
            mk.op("dve", lambda e, j=j: e.bn_stats(out=stats[0:n, j, :], in_=x[0:n, j * 512:(j + 1) * 512]), [xb], [st_b])
        mk.op("dve", lambda e: e.bn_aggr(out=mv[0:n, :], in_=stats[0:n, :, :].rearrange("p a b -> p (a b)")), [st_b], [st_b])
        mk.op("act", lambda e: e.activation(out=sd[0:n, :], in_=mv[0:n, 1:2], func=AF.Sqrt, bias=self.epsln[0:n, :], scale=1.0), [st_b, self.epsb], [st_b])
        mk.op("dve", lambda e: e.reciprocal(out=sd[0:n, :], in_=sd[0:n, :]), [st_b], [st_b])
        mk.op("dve", lambda e: e.tensor_scalar(out=x[0:n, :], in0=x[0:n, :], scalar1=mv[0:n, 0:1], scalar2=sd[0:n, 0:1], op0=ALU.subtract, op1=ALU.mult), [xb, st_b], [xb])
        mk.op("pool", lambda e: e.tensor_tensor(out=x[0:n, :], in0=x[0:n, :], in1=w_bc[0:n, :], op=ALU.mult), [xb, w_bcb], [xb])
        mk.op("dve", lambda e: e.tensor_tensor(out=x[0:n, :], in0=x[0:n, :], in1=b_bc[0:n, :], op=ALU.add), [xb, b_bcb], [xb])

    def proj(self, st, xT, xTb, ntok, W, groups, tok_off, wslots, stg):
        mk = self.mk
        wi = 0
        si = 0
        for g in groups:
            c0, w = g["c0"], g["w"]
            for p0 in range(0, w, 512):
                pw = min(512, w - p0)
                wt, wb = wslots[wi % 2]
                wi += 1
                self.load_w_panel(wt, wb, W, c0 + p0, pw)
                dst, dstb = self.scr[g["dst"]], self.scrb[g["dst"]]
                bf = (dst.dtype == BF16)
                if g["orient"] == "a":
                    for t0 in range(0, ntok, 128):
                        ps, psb = self.bank()
                        for kc in range(KC):
                            mk.op("pe", lambda e, kc=kc: e.matmul(ps[:, 0:pw], xT[:, kc, t0:t0 + 128], wt[:, kc, 0:pw], start=(kc == 0), stop=(kc == KC - 1)),
                                  [xTb, wb], [psb], sig=(kc == KC - 1))
                        sg, sgb = stg[bf][si % 4]
                        si += 1
                        self.evac(g, sg[:, 0:pw], ps[:, 0:pw], psb, sgb)
                        mk.dma("sp", dst[tok_off + t0:tok_off + t0 + 128, g["dc0"] + p0:g["dc0"] + p0 + pw], sg[:, 0:pw], [sgb], [dstb], sgb)
                else:
                    for j0 in range(0, pw, 128):
                        m = min(128, pw - j0)
                        for t0 in range(0, ntok, 512):
                            n = min(512, ntok - t0)
                            ps, psb = self.bank()
                            for kc in range(KC):
                                mk.op("pe", lambda e, kc=kc: e.matmul(ps[0:m, 0:n], wt[:, kc, j0:j0 + m], xT[:, kc, t0:t0 + n], start=(kc == 0), stop=(kc == KC - 1)),
                                      [xTb, wb], [psb], sig=(kc == KC - 1))
                            sg, sgb = stg[bf][si % 4]
                            si += 1
                            self.evac(g, sg[0:m, 0:n], ps[0:m, 0:n], psb, sgb)
                            r0 = g["dc0"] + p0 + j0
                            mk.dma("sp", dst[r0:r0 + m, tok_off + t0:tok_off + t0 + n], sg[0:m, 0:n], [sgb], [dstb], sgb)

    def evac(self, g, out, in_, inb, outb):
        mk = self.mk
        k = g["kind"]
        if k == "copy":
            self.ev = getattr(self, "ev", 0) + 1
            if self.ev % 2:
                mk.op("dve", lambda e: e.tensor_copy(out=out, in_=in_), [inb], [outb])
            else:
                mk.op("act", lambda e: e.copy(out=out, in_=in_), [inb], [outb])
        elif k == "scale":
            mk.op("act", lambda e: e.mul(out, in_, g["scale"]), [inb], [outb])
        elif k == "sigmoid":
            mk.op("act", lambda e: e.activation(out=out, in_=in_, func=AF.Sigmoid), [inb], [outb])
        elif k == "silu":
            mk.op("act", lambda e: e.activation(out=out, in_=in_, func=AF.Silu), [inb], [outb])
        else:
            raise ValueError(k)

    def build_xT(self, st, src, srcb_none, xT, xTb, tok_off, ntok, xin):
        mk = self.mk
        for i, t0 in enumerate(range(0, ntok, 128)):
            xt, xb = xin[i % 2]
            mk.dma("sp", xt[:], src[tok_off + t0:tok_off + t0 + 128, :], [srcb_none] if srcb_none else [], [xb], xb)
            self.transpose_to(xt, xb, lambda j0, n, t0=t0: xT[:, j0:j0 + n, t0:t0 + 128], xTb, KC, False)

    def l0_proj(self):
        mk, nc = self.mk, self.nc
        S = self.dscr
        S("QA_T", [512, F], BF16); S("KA_T", [512, T], BF16); S("VA", [T, 1024], BF16); S("OA", [F, 1024], BF16)
        S("GI_T", [8, T], F32); S("GF_T", [8, T], F32)
        S("QB_T", [1024, F], BF16); S("FF_T", [1024, F], F32); S("FB_T", [1024, T], F32)
        S("IB", [T, 1024], BF16); S("GB", [F, 1024], BF16)
        G = lambda c0, w, o, k, dst, dc0=0, scale=None: dict(c0=c0, w=w, orient=o, kind=k, dst=dst, dc0=dc0, scale=scale)
        g_ka = G(C_KA, 512, "b", "copy", "KA_T")
        g_va = G(C_VA, 1024, "a", "copy", "VA")
        g_gi = G(C_GI, 8, "b", "copy", "GI_T")
        g_gf = G(C_GF, 8, "b", "copy", "GF_T")
        g_fb = G(C_FB, 1024, "b", "copy", "FB_T")
        g_ib = G(C_IB, 1024, "a", "copy", "IB")
        full = [G(C_QA, 512, "b", "scale", "QA_T", scale=128 ** -0.5), g_ka, g_va, G(C_OA, 1024, "a", "sigmoid", "OA"),
                g_gi, g_gf, G(C_QB, 1024, "b", "silu", "QB_T"), G(C_FF, 1024, "b", "copy", "FF_T"), g_fb, g_ib,
                G(C_GB, 1024, "a", "silu", "GB")]
        state = [g_ka, g_va, g_gi, g_gf, g_fb, g_ib]
        with ExitStack() as st:
            xT, xTb = self.sb(st, "xT", [128, KC, F], BF16)
            xin = [self.sb(st, "xin%d" % i, [128, D], F32) for i in range(2)]
            wsl = [self.sb(st, "wp%d" % i, [128, KC, 512], BF16) for i in range(2)]
            stg = {False: [self.sb(st, "sg32_%d" % i, [128, 512], F32) for i in range(4)],
                   True: [self.sb(st, "sg16_%d" % i, [128, 512], BF16) for i in range(4)]}
            self.build_xT(st, self.inp["x"], None, xT, xTb, 0, F, xin)
            self.proj(st, xT, xTb, F, self.inp["w_in"], full, 0, wsl, stg)
            self.build_xT(st, self.inp["x"], None, xT, xTb, F, T - F, xin)
            self.proj(st, xT, xTb, T - F, self.inp["w_in"], state, F, wsl, stg)
            mk.barrier()


def make_consts():
    c = np.zeros((128, 512), np.float32)
    c[:, 256:384] = (np.arange(128)[:, None] < np.arange(128)[None, :])
    c[:, 384:512] = 1.0
    c[:, 0:128] = np.eye(128, dtype=np.float32)
    s = np.arange(64)[:, None]
    t = np.arange(64)[None, :]
    c[0:64, 128:192] = (s <= t)
    c[0:64, 192:256] = (s >= t)
    return c


def win_perm(flip):
    o = {}
    splits = [512, 512, 1024, 1024, 16, 1024, 1024, 1024, 1024, 1024]
    names = ["qa", "ka", "va", "oa", "g", "qb", "ff", "fb", "ib", "gb"]
    c = 0
    for n, s in zip(names, splits):
        o[n] = np.arange(c, c + s)
        c += s
    g = o["g"]
    i_f, f_f, i_b, f_b = g[0:4], g[4:8], g[8:12], g[12:16]
    if flip:
        i_f, f_f, i_b, f_b = i_b, f_b, i_f, f_f
        ff, fb = o["fb"], o["ff"]
    else:
        ff, fb = o["ff"], o["fb"]
    return np.concatenate([o["qa"], o["ka"], o["va"], o["oa"], i_f, i_b, f_f, f_b, o["qb"], ff, fb, o["ib"], o["gb"]])


def _p(cls):
    def deco(f):
        setattr(cls, f.__name__, f)
        return f
    return deco


@_p(Prog)
def small_consts(self, st):
    mk = self.mk
    self.one, self.oneb = self.sb(st, "one", [128, 1], F32)
    self.epsln, self.epsb = self.sb(st, "epsln", [128, 1], F32)
    self.epsrms, self.epsrb = self.sb(st, "epsrms", [128, 1], F32)
    mk.op("dve", lambda e: e.memset(self.one[:], 1.0), [], [self.oneb])
    mk.op("dve", lambda e: e.memset(self.epsln[:], LN_EPS), [], [self.epsb])
    mk.op("dve", lambda e: e.memset(self.epsrms[:], RMS_EPS), [], [self.epsrb])


@_p(Prog)
def l0_gates(self):
    mk = self.mk
    S = self.dscr
    S("EQ", [8, T], F32); S("EK", [8, T], F32); S("EE", [8, T], F32); S("EG", [8, NCT], F32)
    with ExitStack() as st:
        gi, gib = self.sb(st, "gi", [8, T], F32)
        gf, gfb = self.sb(st, "gf", [8, T], F32)
        b_, bb = self.sb(st, "gB", [8, T], F32)
        br, brb = self.sb(st, "gBR", [8, T], F32)
        tmp, tmpb = self.sb(st, "gtmp", [8, T], F32)
        rm, rmb = self.sb(st, "grm", [8, T], F32)
        gbias, gbb = self.sb(st, "gbias_s", [8, 4], F32)
        eg, egb = self.sb(st, "geg", [8, NCT], F32)
        mk.dma("sp", gi[:], self.scr["GI_T"], [self.scrb["GI_T"]], [gib], gib)
        mk.dma("sp", gf[:], self.scr["GF_T"], [self.scrb["GF_T"]], [gfb], gfb)
        mk.dma("sp", rm[:], self.inp["rmask"][0:8, 0:T], [], [rmb], rmb)
        mk.dma("sp", gbias[:, 0:3], self.inp["gbias"], [], [gbb], gbb)
        mk.op("dve", lambda e: e.tensor_scalar(out=gbias[:, 0:2], in0=gbias[:, 0:2], scalar1=1.0 / 15.0, scalar2=None, op0=ALU.mult), [gbb], [gbb])
        mk.op("act", lambda e: e.activation(out=gi[:], in_=gi[:], func=AF.Tanh, bias=gbias[:, 0:1], scale=1.0 / 15.0), [gib, gbb], [gib])
        mk.op("act", lambda e: e.activation(out=gf[:], in_=gf[:], func=AF.Tanh, bias=gbias[:, 1:2], scale=1.0 / 15.0), [gfb, gbb], [gfb])
        mk.op("dve", lambda e: e.tensor_scalar(out=gi[:], in0=gi[:], scalar1=15.0, scalar2=None, op0=ALU.mult), [gib], [gib])
        mk.op("act", lambda e: e.activation(out=gf[:], in_=gf[:], func=AF.Exp, scale=-15.0), [gfb], [gfb])
        mk.op("act", lambda e: e.activation(out=gf[:], in_=gf[:], func=AF.Ln, bias=self.one[0:8, :], scale=1.0), [gfb, self.oneb], [gfb])
        mk.op("dve", lambda e: e.tensor_scalar(out=gf[:], in0=gf[:], scalar1=-1.0, scalar2=None, op0=ALU.mult), [gfb], [gfb])
        mk.op("dve", lambda e: e.tensor_tensor_scan(out=b_[:], data0=rm[:], data1=gf[:], initial=0.0, op0=ALU.mult, op1=ALU.add), [rmb, gfb], [bb])
        v3 = lambda t: t[:].rearrange("p (c l) -> p c l", l=CH)
        mk.op("dve", lambda e: e.tensor_copy(out=eg[:].rearrange("p (c o) -> p c o", o=1), in_=v3(b_)[:, :, CH - 1:CH]), [bb], [egb])
        gbc = eg[:].rearrange("p (c o) -> p c o", o=1).to_broadcast([8, NCT, CH])
        mk.op("dve", lambda e: e.tensor_tensor(out=v3(br), in0=gbc, in1=v3(b_), op=ALU.subtract), [egb, bb], [brb])
        mk.op("dve", lambda e: e.tensor_tensor(out=br[:], in0=br[:], in1=gf[:], op=ALU.add), [brb, gfb], [brb])
        mk.op("dve", lambda e: e.tensor_tensor(out=tmp[:], in0=b_[:], in1=br[:], op=ALU.subtract), [bb, brb], [tmpb])
        mk.op("dve", lambda e: e.scalar_tensor_tensor(out=b_[:], in0=tmp[:], scalar=gbias[:, 2:3], in1=br[:], op0=ALU.mult, op1=ALU.add), [tmpb, brb, gbb], [bb])
        mk.op("act", lambda e: e.activation(out=tmp[:], in_=b_[:], func=AF.Exp), [bb], [tmpb])
        mk.dma("sp", self.scr["EQ"], tmp[:], [tmpb], [self.scrb["EQ"]], tmpb)
        mk.op("dve", lambda e: e.tensor_tensor(out=br[:], in0=gi[:], in1=b_[:], op=ALU.subtract), [gib, bb], [brb])
        mk.op("act", lambda e: e.activation(out=br[:], in_=br[:], func=AF.Exp), [brb], [brb])
        mk.dma("sp", self.scr["EK"], br[:], [brb], [self.scrb["EK"]], brb)
        mk.op("act", lambda e: e.activation(out=eg[:], in_=eg[:], func=AF.Exp), [egb], [egb])
        mk.dma("sp", self.scr["EG"], eg[:], [egb], [self.scrb["EG"]], egb)
        mk.op("dve", lambda e: e.tensor_tensor(out=v3(gi), in0=v3(br), in1=gbc, op=ALU.mult), [brb, egb], [gib])
        mk.dma("sp", self.scr["EE"], gi[:], [gib], [self.scrb["EE"]], gib)
        mk.barrier()


@_p(Prog)
def rec_pass1(self, chains, V, Vb, dvp):
    mk = self.mk
    GP = 8
    for ci, ch in enumerate(chains):
        mk.op("dve", lambda e: e.memset(ch["S"][:, 0:dvp], 0.0), [], [ch["Sb"]])
        ch["groups"] = [ch["order"][k:k + GP] for k in range(0, len(ch["order"]), GP)]
        ch["bank"] = (self.pst, self.pstall) if ci == 0 else (self.pst2, [self.pst2b])
    ng = max(len(ch["groups"]) for ch in chains)
    for g in range(ng + 1):
        for ch in chains:
            if g < len(ch["groups"]):
                grp = ch["groups"][g]
                bk, bkb = ch["bank"]
                for k, c in enumerate(grp):
                    mk.op("pe", lambda e: e.transpose(bk[0:CH, k * 128:(k + 1) * 128], ch["kend"][:, c * CH:(c + 1) * CH], self.identh[:]),
                          [ch["kendb"], self.identhb], bkb, sig=(k == len(grp) - 1))
                kt, ktb = ch["kts"][g % 2]
                n = len(grp)
                mk.op("act", lambda e: e.copy(out=kt[:, 0:n, :], in_=bk[0:CH, 0:n * 128].rearrange("p (j t) -> p j t", t=128)), bkb, [ktb])
        if g >= 1:
            for k in range(GP):
                for ch in chains:
                    if g - 1 < len(ch["groups"]) and k < len(ch["groups"][g - 1]):
                        c = ch["groups"][g - 1][k]
                        kt, ktb = ch["kts"][(g - 1) % 2]
                        ps, psb = self.bank(NBK)
                        S, Sb = ch["S"], ch["Sb"]
                        mk.op("pe", lambda e: e.matmul(ps[:, 0:dvp], kt[:, k, :], V[:, c, 0:dvp], start=True, stop=True), [ktb, Vb], [psb])
                        mk.op("dve", lambda e: e.scalar_tensor_tensor(out=S[:, 0:dvp], in0=S[:, 0:dvp], scalar=ch["decay"][:, c:c + 1], in1=ps[:, 0:dvp], op0=ALU.mult, op1=ALU.add),
                              [Sb, ch["decayb"], psb], [Sb])
                        s_ = ch["store"](c)
                        if s_ is not None:
                            mk.op("act", lambda e: e.copy(out=ch["Sbf"][:, s_, 0:dvp], in_=S[:, 0:dvp]), [Sb], [ch["Sbfb"]])


@_p(Prog)
def rec_scores2(self, c, kin, qin, masks, pts, i):
    mk = self.mk
    sl = slice(c * CH, (c + 1) * CH)
    out = []
    for d in range(2):
        ps, psb = self.bank(NBK)
        mk.op("pe", lambda e: e.matmul(ps[0:CH, 0:CH], kin[d][0][:, sl], qin[d][0][:, sl], start=True, stop=True), [kin[d][1], qin[d][1]], [psb])
        pt, ptb = pts[(2 * i + d) % len(pts)]
        mk.op("dve", lambda e: e.tensor_tensor(out=pt[:], in0=ps[0:CH, 0:CH], in1=masks[d][0][:], op=ALU.mult), [psb, masks[d][1]], [ptb])
        out.append((pt, ptb))
    return out


@_p(Prog)
def l0_mlstm(self):
    mk = self.mk
    if "Y" not in self.scr:
        self.dscr("Y", [F, D], BF16)
    Y, Yb = self.scr["Y"], self.scrb["Y"]
    dvp = 257
    with ExitStack() as st:
        qraw, qrawb = self.sb(st, "m_qraw", [128, F], BF16)
        kraw, krawb = self.sb(st, "m_kraw", [128, T], BF16)
        bc, bcb = self.sb(st, "m_bc", [128, T], F32)
        qin = [self.sb(st, "m_qin%d" % d, [128, F], BF16) for d in range(2)]
        kin = [self.sb(st, "m_kin%d" % d, [128, F], BF16) for d in range(2)]
        kend = [self.sb(st, "m_kend0", [128, F], BF16), self.sb(st, "m_kend1", [128, T], BF16)]
        decay = [self.sb(st, "m_dec%d" % d, [128, NCT], F32) for d in range(2)]
        V, Vb = self.sb(st, "m_V", [CH, NCT, dvp], BF16)
        Sbf = [self.sb(st, "m_Sbf%d" % d, [128, NCF + 1, dvp], BF16) for d in range(2)]
        S2 = [self.sb(st, "m_S%d" % i, [128, dvp], F32) for i in range(2)]
        kts = [self.sb(st, "m_kt%d" % i, [CH, 8, 128], BF16) for i in range(4)]
        pts = [self.sb(st, "m_pt%d" % i, [CH, CH], BF16) for i in range(8)]
        oaw, oawb = self.sb(st, "m_oaw", [CH, NCF, 256], BF16)
        wbc, wbcb = self.sb(st, "m_wbc", [CH, 256], F32)
        Yh, Yhb = self.sb(st, "m_Yh", [CH, NCF, 256], BF16)
        hs = [self.sb(st, "m_hs%d" % i, [CH, 256], F32) for i in range(3)]
        junk, junkb = self.sb(st, "m_junk", [CH, 256], F32)
        sm = [self.sb(st, "m_sm%d" % i, [CH, 8], F32) for i in range(4)]
        masks = [(self.mut, self.mutb), (self.mlt, self.mltb)]
        for h in range(4):
            mk.dma("sp", qraw[:], self.scr["QA_T"][h * 128:(h + 1) * 128, :], [self.scrb["QA_T"]], [qrawb], qrawb)
            mk.dma("sp", kraw[:], self.scr["KA_T"][h * 128:(h + 1) * 128, :], [self.scrb["KA_T"]], [krawb], krawb)
            mk.dma("sp", V[:, :, 0:256], self.scr["VA"][:, h * 256:(h + 1) * 256].rearrange("(c p) e -> p c e", p=CH), [self.scrb["VA"]], [Vb], Vb)
            mk.op("dve", lambda e: e.memset(V[:, :, 256:257], 1.0), [], [Vb])
            mk.dma("sp", oaw[:], self.scr["OA"][:, h * 256:(h + 1) * 256].rearrange("(c p) e -> p c e", p=CH), [self.scrb["OA"]], [oawb], oawb)
            mk.dma("sp", wbc[:], self.inp["mlstm_norm_w"][h:h + 1, :].to_broadcast([CH, 256]), [], [wbcb], wbcb)
            mk.op("dve", lambda e: e.tensor_tensor(out=oaw[:], in0=oaw[:], in1=wbc[:].rearrange("p (o e) -> p o e", o=1).to_broadcast([CH, NCF, 256]), op=ALU.mult), [oawb, wbcb], [oawb])
            for d in range(2):
                r = d * 4 + h
                Td = F if d == 0 else T
                for (name, dst, src, n) in (("EQ", qin[d], qraw, F), ("EK", kin[d], kraw, F), ("EE", kend[d], kraw, Td)):
                    mk.dma("sp", bc[:, 0:n], self.scr[name][r:r + 1, 0:n].to_broadcast([128, n]), [self.scrb[name]], [bcb], bcb)
                    mk.op("dve", lambda e, dst=dst, src=src, n=n: e.tensor_tensor(out=dst[0][:, 0:n], in0=src[:, 0:n], in1=bc[:, 0:n], op=ALU.mult),
                          [bcb, qrawb, krawb], [dst[1]])
                mk.dma("sp", decay[d][0][:], self.scr["EG"][r:r + 1, :].to_broadcast([128, NCT]), [self.scrb["EG"]], [decay[d][1]], decay[d][1])
            chains = [dict(kend=kend[0][0], kendb=kend[0][1], decay=decay[0][0], decayb=decay[0][1], S=S2[0][0], Sb=S2[0][1], Sbf=Sbf[0][0], Sbfb=Sbf[0][1],
                           kts=kts[0:2], order=list(range(NCF - 1)), store=lambda c: c + 1, slot0=0),
                      dict(kend=kend[1][0], kendb=kend[1][1], decay=decay[1][0], decayb=decay[1][1], S=S2[1][0], Sb=S2[1][1], Sbf=Sbf[1][0], Sbfb=Sbf[1][1],
                           kts=kts[2:4], order=list(range(NCT - 1, 0, -1)), store=lambda c: (c - 1) if c <= NCF else None, slot0=4)]
            self.rec_pass1(chains, V, Vb, dvp)
            stA, stO = {}, {}
            for it in range(NCF + SK_E):
                c = it
                if c < NCF:
                    stA[c] = self.rec_scores2(c, kin, qin, masks, pts, c)
                c = it - SK_O
                if 0 <= c < NCF:
                    sl = slice(c * CH, (c + 1) * CH)
                    pso = []
                    for d in range(2):
                        pt, ptb = stA[c][d]
                        ps, psb = self.bank(NBK)
                        has_state = (c > 0) if d == 0 else True
                        mk.op("pe", lambda e: e.matmul(ps[0:CH, 0:dvp], pt[:], V[:, c, :], start=True, stop=not has_state), [ptb, Vb], [psb], sig=not has_state)
                        if has_state:
                            mk.op("pe", lambda e: e.matmul(ps[0:CH, 0:dvp], qin[d][0][:, sl], Sbf[d][0][:, c, :], start=False, stop=True), [qin[d][1], Sbf[d][1]], [psb])
                        pso.append((ps, psb))
                    hsT, hsb = hs[c % 3]
                    s_, s_b = sm[c % 4]
                    for d in range(2):
                        ps, psb = pso[d]
                        o = 4 * d
                        mk.op("dve", lambda e: e.tensor_scalar(out=s_[:, o + 3:o + 4], in0=ps[0:CH, 256:257], scalar1=-1.0, scalar2=1.0, op0=ALU.mult, op1=ALU.max), [psb], [s_b])
                        mk.op("dve", lambda e: e.scalar_tensor_tensor(out=s_[:, o:o + 1], in0=ps[0:CH, 256:257], scalar=1.0, in1=s_[:, o + 3:o + 4], op0=ALU.max, op1=ALU.max), [psb, s_b], [s_b])
                        mk.op("dve", lambda e: e.reciprocal(out=s_[:, o:o + 1], in_=s_[:, o:o + 1]), [s_b], [s_b])
                        if d == 0:
                            mk.op("act", lambda e: e.activation(out=hsT[:], in_=ps[0:CH, 0:256], func=AF.Copy, scale=s_[:, o:o + 1]), [psb, s_b], [hsb])
                        else:
                            mk.op("dve", lambda e: e.scalar_tensor_tensor(out=hsT[:], in0=ps[0:CH, 0:256], scalar=s_[:, o:o + 1], in1=hsT[:], op0=ALU.mult, op1=ALU.add), [psb, s_b, hsb], [hsb])
                    mk.op("act", lambda e: e.activation(out=junk[:], in_=hsT[:], func=AF.Square, accum_out=s_[:, 1:2]), [hsb], [junkb, s_b])
                    mk.op("act", lambda e: e.activation(out=s_[:, 2:3], in_=s_[:, 1:2], func=AF.Sqrt, bias=self.epsrms[0:CH, :], scale=1.0 / 256.0), [s_b, self.epsrb], [s_b])
                c = it - SK_E
                if 0 <= c < NCF:
                    hsT, hsb = hs[c % 3]
                    s_, s_b = sm[c % 4]
                    mk.op("dve", lambda e: e.reciprocal(out=s_[:, 2:3], in_=s_[:, 2:3]), [s_b], [s_b])
                    mk.op("dve", lambda e: e.scalar_tensor_tensor(out=Yh[:, c, :], in0=hsT[:], scalar=s_[:, 2:3], in1=oaw[:, c, :], op0=ALU.mult, op1=ALU.mult), [hsb, s_b, oawb], [Yhb])
            mk.dma("sp", Y[:, h * 256:(h + 1) * 256].rearrange("(c p) e -> p c e", p=CH), Yh[:], [Yhb], [Yb], Yhb)
        mk.barrier()


@_p(Prog)
def l0_hgrn(self):
    mk = self.mk
    if "Y" not in self.scr:
        self.dscr("Y", [F, D], BF16)
    Y, Yb = self.scr["Y"], self.scrb["Y"]
    dvp = 128
    v3 = lambda ap: ap.rearrange("p (c l) -> p c l", l=CH)
    REV = False
    with ExitStack() as st:
        A, Ab = self.sb(st, "h_A", [128, T], F32)
        B, Bb = self.sb(st, "h_B", [128, T], F32)
        C, Cb = self.sb(st, "h_C", [128, T], F32)
        rm, rmb = self.sb(st, "h_rm", [128, T + CH], F32)
        Abq = [mk.buf("h_Aq%d" % i) for i in range(4)]
        Bbq = [mk.buf("h_Bq%d" % i) for i in range(4)]
        Cbq = [mk.buf("h_Cq%d" % i) for i in range(4)]
        qs, qsb = self.sb(st, "h_qs", [128, F], BF16)
        qin = [self.sb(st, "h_qin%d" % d, [128, F], BF16) for d in range(2)]
        kin = [self.sb(st, "h_kin0", [128, F], BF16), self.sb(st, "h_kin1", [128, T], BF16)]
        kend = [self.sb(st, "h_kend0", [128, F], BF16), self.sb(st, "h_kend1", [128, T], BF16)]
        decay = [self.sb(st, "h_dec%d" % d, [128, NCT], F32) for d in range(2)]
        V, Vb = self.sb(st, "h_V", [CH, NCT, dvp], BF16)
        Sbf = [self.sb(st, "h_Sbf%d" % d, [128, NCF + 1, dvp], BF16) for d in range(2)]
        S2 = [self.sb(st, "h_S%d" % i, [128, dvp], F32) for i in range(2)]
        kts = [self.sb(st, "h_kt%d" % i, [CH, 8, 128], BF16) for i in range(4)]
        pts = [self.sb(st, "h_pt%d" % i, [CH, CH], BF16) for i in range(8)]
        gbw, gbwb = self.sb(st, "h_gbw", [CH, NCF, 128], BF16)
        wbc, wbcb = self.sb(st, "h_wbc", [CH, 128], F32)
        Yhs = [self.sb(st, "h_Yh%d" % i, [CH, NCF, 128], BF16) for i in range(2)]
        junk, junkb = self.sb(st, "h_junk", [CH, 128], F32)
        sm = [self.sb(st, "h_sm%d" % i, [CH, 4], F32) for i in range(4)]
        lbl, lblb = self.sb(st, "h_lbl", [128, 32], F32)
        LB, LBb = self.sb(st, "h_LB", [128, 16], F32)
        OML, OMLb = self.sb(st, "h_OML", [128, 16], F32)
        masks = [(self.mut, self.mutb), (self.mlt, self.mltb)]
        mk.dma("sp", rm[:], self.inp["rmask"], [], [rmb], rmb)
        mk.dma("sp", lbl[:], self.inp["lbl"], [], [lblb], lblb)
        l4 = lbl[:].rearrange("p (d s h) -> p d s h", d=2, s=2)
        mk.op("dve", lambda e: e.tensor_tensor(out=LB[:].rearrange("p (d h) -> p d h", d=2), in0=l4[:, :, 0, :], in1=l4[:, :, 1, :], op=ALU.subtract), [lblb], [LBb])
        mk.op("act", lambda e: e.activation(out=LB[:], in_=LB[:], func=AF.Sigmoid), [LBb], [LBb])
        mk.op("dve", lambda e: e.tensor_scalar(out=OML[:], in0=LB[:], scalar1=-1.0, scalar2=1.0, op0=ALU.mult, op1=ALU.add), [LBb], [OMLb])
        for h in range(8):
            rows = slice(h * 128, (h + 1) * 128)
            Yh, Yhb = Yhs[h % 2]
            mk.dma("sp", qs[:], self.scr["QB_T"][rows, :], [self.scrb["QB_T"]], [qsb], qsb)
            mk.dma("sp", V[:], self.scr["IB"][:, rows].rearrange("(c p) e -> p c e", p=CH), [self.scrb["IB"]], [Vb], Vb)
            mk.dma("sp", gbw[:], self.scr["GB"][:, rows].rearrange("(c p) e -> p c e", p=CH), [self.scrb["GB"]], [gbwb], gbwb)
            mk.dma("sp", wbc[:], self.inp["hgrn_norm_w"][h:h + 1, :].to_broadcast([CH, 128]), [], [wbcb], wbcb)
            mk.op("pool", lambda e: e.tensor_tensor(out=gbw[:], in0=gbw[:], in1=wbc[:].rearrange("p (o e) -> p o e", o=1).to_broadcast([CH, NCF, 128]), op=ALU.mult), [gbwb, wbcb], [gbwb])
            for d in range(2):
                Td = F if d == 0 else T
                nc_ = Td // CH
                src = "FF_T" if d == 0 else "FB_T"
                col = d * 8 + h
                omc, lbc = OML[:, col:col + 1], LB[:, col:col + 1]
                dec, decb = decay[d]
                blks = [(k, k * 1024, min((k + 1) * 1024, Td)) for k in range(4) if k * 1024 < Td]
                for k, s0, e0 in blks:
                    mk.dma("sp", A[:, s0:e0], self.scr[src][rows, s0:e0], [self.scrb[src]], [Abq[k]], Abq[k])
                for k, s0, e0 in blks:
                    mk.op("act", lambda e: e.activation(out=B[:, s0:e0], in_=A[:, s0:e0], func=AF.Sigmoid), [Abq[k]], [Bbq[k]])
                for k, s0, e0 in blks:
                    mk.op("act", lambda e: e.activation(out=A[:, s0:e0], in_=A[:, s0:e0], func=AF.Sigmoid, scale=-1.0), [Abq[k]], [Abq[k]])
                for k, s0, e0 in blks:
                    mk.op("act", lambda e: e.activation(out=B[:, s0:e0], in_=B[:, s0:e0], func=AF.Ln, scale=omc, bias=lbc), [Bbq[k], OMLb, LBb], [Bbq[k]])
                for k, s0, e0 in blks:
                    c0, c1 = s0 // CH, e0 // CH
                    mk.op("dve", lambda e: e.tensor_tensor_scan(out=C[:, s0:e0], data0=rm[:, s0:e0], data1=B[:, s0:e0], initial=0.0, op0=ALU.mult, op1=ALU.add), [rmb, Bbq[k]], [Cbq[k]])
                    mk.op("dve", lambda e: e.tensor_copy(out=dec[:, c0:c1].rearrange("p (c o) -> p c o", o=1), in_=v3(C[:, s0:e0])[:, :, CH - 1:CH]), [Cbq[k]], [decb])
                    if d == 1:
                        gb_ = dec[:, c0:c1].rearrange("p (c o) -> p c o", o=1).to_broadcast([128, c1 - c0, CH])
                        mk.op("dve", lambda e: e.scalar_tensor_tensor(out=v3(C[:, s0:e0]), in0=v3(C[:, s0:e0]), scalar=-1.0, in1=gb_, op0=ALU.mult, op1=ALU.add), [Cbq[k], decb], [Cbq[k]])
                        mk.op("pool", lambda e: e.tensor_tensor(out=C[:, s0:e0], in0=C[:, s0:e0], in1=B[:, s0:e0], op=ALU.add), [Cbq[k], Bbq[k]], [Cbq[k]])
                for k, s0, e0 in blks:
                    mk.op("act", lambda e: e.activation(out=B[:, s0:e0], in_=C[:, s0:e0], func=AF.Exp, scale=-1.0), [Cbq[k], Bbq[k]], [Bbq[k]])
                mk.op("act", lambda e: e.activation(out=dec[:, 0:nc_], in_=dec[:, 0:nc_], func=AF.Exp), [decb], [decb])
                for k, s0, e0 in blks:
                    c0, c1 = s0 // CH, e0 // CH
                    mk.op("dve", lambda e: e.scalar_tensor_tensor(out=kin[d][0][:, s0:e0], in0=A[:, s0:e0], scalar=omc, in1=B[:, s0:e0], op0=ALU.mult, op1=ALU.mult), [Abq[k], Bbq[k], OMLb], [kin[d][1]])
                    egb = dec[:, c0:c1].rearrange("p (c o) -> p c o", o=1).to_broadcast([128, c1 - c0, CH])
                    mk.op("pool", lambda e: e.tensor_tensor(out=v3(kend[d][0][:, s0:e0]), in0=v3(kin[d][0][:, s0:e0]), in1=egb, op=ALU.mult), [kin[d][1], decb], [kend[d][1]])
                for k, s0, e0 in blks:
                    e1 = min(e0, F)
                    if s0 < e1:
                        mk.op("act", lambda e: e.activation(out=A[:, s0:e1], in_=C[:, s0:e1], func=AF.Exp), [Cbq[k], Abq[k]], [Abq[k]])
                        mk.op("dve", lambda e: e.tensor_tensor(out=qin[d][0][:, s0:e1], in0=qs[:, s0:e1], in1=A[:, s0:e1], op=ALU.mult), [qsb, Abq[k]], [qin[d][1]])
            chains = [dict(kend=kend[0][0], kendb=kend[0][1], decay=decay[0][0], decayb=decay[0][1], S=S2[0][0], Sb=S2[0][1], Sbf=Sbf[0][0], Sbfb=Sbf[0][1],
                           kts=kts[0:2], order=list(range(NCF - 1)), store=lambda c: c + 1, slot0=0),
                      dict(kend=kend[1][0], kendb=kend[1][1], decay=decay[1][0], decayb=decay[1][1], S=S2[1][0], Sb=S2[1][1], Sbf=Sbf[1][0], Sbfb=Sbf[1][1],
                           kts=kts[2:4], order=list(range(NCT - 1, 0, -1)), store=lambda c: (c - 1) if c <= NCF else None, slot0=4)]
            self.rec_pass1(chains, V, Vb, dvp)
            stA, stO = {}, {}
            for it in range(NCF + SK_E):
                c = it
                if c < NCF:
                    stA[c] = self.rec_scores2(c, kin, qin, masks, pts, c)
                c = it - SK_O
                if 0 <= c < NCF:
                    sl = slice(c * CH, (c + 1) * CH)
                    ptl = stA[c]
                    ps, psb = self.bank(NBK)
                    mk.op("pe", lambda e: e.matmul(ps[0:CH, 0:dvp], ptl[0][0][:], V[:, c, :], start=True, stop=False), [ptl[0][1], Vb], [psb], sig=False)
                    mk.op("pe", lambda e: e.matmul(ps[0:CH, 0:dvp], ptl[1][0][:], V[:, c, :], start=False, stop=False), [ptl[1][1], Vb], [psb], sig=False)
                    if c > 0:
                        mk.op("pe", lambda e: e.matmul(ps[0:CH, 0:dvp], qin[0][0][:, sl], Sbf[0][0][:, c, :], start=False, stop=False), [qin[0][1], Sbf[0][1]], [psb], sig=False)
                    mk.op("pe", lambda e: e.matmul(ps[0:CH, 0:dvp], qin[1][0][:, sl], Sbf[1][0][:, c, :], start=False, stop=True), [qin[1][1], Sbf[1][1]], [psb])
                    stO[c] = (ps, psb)
                    s_, s_b = sm[c % 4]
                    mk.op("act", lambda e: e.activation(out=junk[:], in_=ps[0:CH, 0:dvp], func=AF.Square, accum_out=s_[:, 1:2]), [psb], [junkb, s_b])
                    mk.op("act", lambda e: e.activation(out=s_[:, 2:3], in_=s_[:, 1:2], func=AF.Sqrt, bias=self.epsrms[0:CH, :], scale=1.0 / 128.0), [s_b, self.epsrb], [s_b])
                c = it - SK_E
                if 0 <= c < NCF:
                    ps, psb = stO[c]
                    s_, s_b = sm[c % 4]
                    mk.op("dve", lambda e: e.reciprocal(out=s_[:, 2:3], in_=s_[:, 2:3]), [s_b], [s_b])
                    mk.op("dve", lambda e: e.scalar_tensor_tensor(out=Yh[:, c, :], in0=ps[0:CH, 0:dvp], scalar=s_[:, 2:3], in1=gbw[:, c, :], op0=ALU.mult, op1=ALU.mult), [psb, s_b, gbwb], [Yhb])
            mk.dma("pool", Y[:, 1024 + h * 128:1024 + (h + 1) * 128].rearrange("(c p) e -> p c e", p=CH), Yh[:], [Yhb], [Yb], Yhb)
        mk.barrier()


def make_rmask():
    r = np.ones((128, T + CH), np.float32)
    r[:, ::CH] = 0.0
    return r


def make_gbias(gb, flip):
    i_f, f_f, i_b, f_b = gb[0], gb[1], gb[2], gb[3]
    if flip:
        i_f, f_f, i_b, f_b = i_b, f_b, i_f, f_f
    out = np.zeros((8, 3), np.float32)
    out[:, 0] = np.concatenate([i_f, i_b])
    out[:, 1] = np.concatenate([f_f, f_b])
    out[0:4, 2] = 1.0
    return out


def make_lbl(lg, flip):
    a = lg[::-1] if flip else lg
    a = a.reshape(2, 2, 8, 128).transpose(3, 0, 1, 2).reshape(128, 32)
    return np.ascontiguousarray(a)


@_p(Prog)
def load_ln(self, st, i, k, tag):
    w_bc, w_bcb = self.sb(st, tag + "_lnw", [128, D], F32)
    b_bc, b_bcb = self.sb(st, tag + "_lnb", [128, D], F32)
    self.mk.dma("sp", w_bc[:], self.inp["ln_w"][2 * i + k:2 * i + k + 1, :].to_broadcast([128, D]), [], [w_bcb], w_bcb)
    self.mk.dma("sp", b_bc[:], self.inp["ln_b"][2 * i + k:2 * i + k + 1, :].to_broadcast([128, D]), [], [b_bcb], b_bcb)
    stats = self.sb(st, tag + "_st", [128, 4, 6], F32)
    mv = self.sb(st, tag + "_mv", [128, 2], F32)
    sd = self.sb(st, tag + "_sd", [128, 1], F32)
    return (w_bc, w_bcb, b_bc, b_bcb, (stats[0], mv[0], sd[0]), stats[1])


@_p(Prog)
def preload_w(self, st, tag, W):
    wres, wresb = self.sb(st, tag + "_w", [128, KC, D], BF16)
    for c0 in range(0, D, 512):
        self.mk.dma("pool", wres[:, :, c0:c0 + 512], W[:, c0:c0 + 512].rearrange("(kc p) n -> p kc n", p=128), [], [wresb], wresb)
    return wres, wresb


@_p(Prog)
def mix_ln(self, tag, ysrc, W, resid, residb, li, ntok, dst, pre=None):
    mk = self.mk
    Ys, Ysb = self.scr[ysrc], self.scrb[ysrc]
    with ExitStack() as st:
        wres, wresb = pre if pre is not None else self.preload_w(st, tag, W)
        w_bc, w_bcb, b_bc, b_bcb, stt, stb = self.load_ln(st, li, 0, tag)
        yt = [self.sb(st, tag + "_y%d" % i, [128, D], BF16) for i in range(3)]
        yT = [self.sb(st, tag + "_yT%d" % i, [128, KC, 128], BF16) for i in range(3)]
        xr = [self.sb(st, tag + "_x%d" % i, [128, D], F32) for i in range(3)]
        def loads(i):
            t0 = i * 128
            mk.dma("sp", yt[i % 3][0][:], Ys[t0:t0 + 128, :], [Ysb], [yt[i % 3][1]], yt[i % 3][1])
            mk.dma("sp", xr[i % 3][0][:], resid[t0:t0 + 128, :], [residb] if residb else [], [xr[i % 3][1]], xr[i % 3][1])
        loads(0)
        for i, t0 in enumerate(range(0, ntok, 128)):
            y, yb = yt[i % 3]
            yTt, yTb = yT[i % 3]
            x, xb = xr[i % 3]
            if t0 + 128 < ntok:
                loads(i + 1)
            self.transpose_to(y, yb, lambda j0, n: yTt[:, j0:j0 + n, :], yTb, KC, True)
            for cb in range(4):
                ps, psb = self.bank()
                for kc in range(KC):
                    mk.op("pe", lambda e: e.matmul(ps[:, :], yTt[:, kc, :], wres[:, kc, cb * 512:(cb + 1) * 512], start=(kc == 0), stop=(kc == KC - 1)),
                          [yTb, wresb], [psb], sig=(kc == KC - 1))
                mk.op("dve", lambda e: e.scalar_tensor_tensor(out=x[:, cb * 512:(cb + 1) * 512], in0=x[:, cb * 512:(cb + 1) * 512], scalar=ALPHA, in1=ps[:, :], op0=ALU.mult, op1=ALU.add),
                      [xb, psb], [xb])
            self.layer_norm(x, xb, stt, stb, w_bc, w_bcb, b_bc, b_bcb)
            mk.dma("sp", self.scr[dst][t0:t0 + 128, :], x[:], [xb], [self.scrb[dst]], xb)
        mk.barrier()


@_p(Prog)
def ffn_dense(self, src, ntok, dst):
    mk = self.mk
    Wg, Wu, Wd = self.inp["ffn_w_gate"], self.inp["ffn_w_up"], self.inp["ffn_w_down"]
    NH = FFN // 128
    TS = 768
    NB = 384
    with ExitStack() as st:
        xaT, xaTb = self.sb(st, "f_xT", [128, KC, TS], BF16)
        hT, hTb = self.sb(st, "f_hT", [128, NH, TS], BF16)
        acc = [self.sb(st, "f_acc%d" % i, [128, D], F32) for i in range(TS // 128)]
        wg = [self.sb(st, "f_wg%d" % i, [128, KC, 128], BF16) for i in range(2)]
        wu = [self.sb(st, "f_wu%d" % i, [128, KC, 128], BF16) for i in range(2)]
        wd = [self.sb(st, "f_wd%d" % i, [128, 8, 512], BF16) for i in range(2)]
        sg = [self.sb(st, "f_sg%d" % i, [128, NB], F32) for i in range(2)]
        na = 0
        for s0 in range(0, ntok, TS):
            n = min(TS, ntok - s0)
            ntile = n // 128
            for tt in range(ntile):
                a, ab = acc[tt]
                mk.dma("sp", a[:], self.scr[src][s0 + tt * 128:s0 + (tt + 1) * 128, :], [self.scrb[src]], [ab], ab)
                self.transpose_to(a, ab, lambda j0, m, tt=tt: xaT[:, j0:j0 + m, tt * 128:(tt + 1) * 128], xaTb, KC, False)
                mk.op("pool", lambda e: e.tensor_scalar(out=a[:], in0=a[:], scalar1=ALPHA, scalar2=None, op0=ALU.mult), [ab], [ab])
            for j in range(NH):
                g_, gb_ = wg[j % 2]
                u_, ub_ = wu[j % 2]
                self.load_w_panel(g_, gb_, Wg, j * 128, 128)
                self.load_w_panel(u_, ub_, Wu, j * 128, 128)
                for b0 in range(0, n, NB):
                    nb = min(NB, n - b0)
                    bs = slice(b0, b0 + nb)
                    psg, psgb = self.bank()
                    psu, psub = self.bank()
                    for kc in range(KC):
                        mk.op("pe", lambda e: e.matmul(psg[:, 0:nb], g_[:, kc, :], xaT[:, kc, bs], start=(kc == 0), stop=(kc == KC - 1)), [gb_, xaTb], [psgb], sig=(kc == KC - 1))
                    for kc in range(KC):
                        mk.op("pe", lambda e: e.matmul(psu[:, 0:nb], u_[:, kc, :], xaT[:, kc, bs], start=(kc == 0), stop=(kc == KC - 1)), [ub_, xaTb], [psub], sig=(kc == KC - 1))
                    s_, s_b = sg[(b0 // NB) % 2]
                    mk.op("act", lambda e: e.activation(out=s_[:, 0:nb], in_=psg[:, 0:nb], func=AF.Silu), [psgb], [s_b])
                    mk.op("dve", lambda e: e.tensor_tensor(out=hT[:, j, bs], in0=s_[:, 0:nb], in1=psu[:, 0:nb], op=ALU.mult), [s_b, psub], [hTb])
            pi = 0
            for cb in range(4):
                cs = slice(cb * 512, (cb + 1) * 512)
                for j0 in range(0, NH, 8):
                    nj = min(8, NH - j0)
                    d_, db_ = wd[pi % 2]
                    pi += 1
                    mk.dma("pool", d_[:, 0:nj, :], Wd[j0 * 128:(j0 + nj) * 128, cs].rearrange("(j p) n -> p j n", p=128), [], [db_], db_)
                    for tt in range(ntile):
                        a, ab = acc[tt]
                        ps, psb = self.bank()
                        for jj in range(nj):
                            mk.op("pe", lambda e: e.matmul(ps[:, :], hT[:, j0 + jj, tt * 128:(tt + 1) * 128], d_[:, jj, :], start=(jj == 0), stop=(jj == nj - 1)), [hTb, db_], [psb], sig=(jj == nj - 1))
                        mk.op("dve", lambda e: e.tensor_tensor(out=a[:, cs], in0=a[:, cs], in1=ps[:, :], op=ALU.add), [ab, psb], [ab])
            for tt in range(ntile):
                a, ab = acc[tt]
                mk.dma("sp", self.scr[dst][s0 + tt * 128:s0 + (tt + 1) * 128, :], a[:], [ab], [self.scrb[dst]], ab)
        mk.barrier()


@_p(Prog)
def ln_ple(self, tag, src, li, ntok, dst_ap, dstb, p_ap, pre=None):
    mk = self.mk
    Wpg = self.inp["ple_w_gate"][li * D:(li + 1) * D, :]
    Wpp = self.inp["ple_w_proj"][li * 256:(li + 1) * 256, :]
    with ExitStack() as st:
        wres, wresb = pre if pre is not None else self.preload_w(st, tag, Wpg)
        wpp, wppb = self.sb(st, tag + "_wpp", [128, 2, D], BF16)
        for c0 in range(0, D, 1024):
            mk.dma("pool", wpp[:, :, c0:c0 + 1024], Wpp[:, c0:c0 + 1024].rearrange("(kc p) n -> p kc n", p=128), [], [wppb], wppb)
        w_bc, w_bcb, b_bc, b_bcb, stt, stb = self.load_ln(st, li, 1, tag)
        xr = [self.sb(st, tag + "_x%d" % i, [128, D], F32) for i in range(3)]
        xT = [self.sb(st, tag + "_xT%d" % i, [128, KC, 128], BF16) for i in range(3)]
        pin = [self.sb(st, tag + "_p%d" % i, [128, 256], F32) for i in range(3)]
        pT = [self.sb(st, tag + "_pT%d" % i, [128, 2, 128], BF16) for i in range(3)]
        sg = [self.sb(st, tag + "_sg%d" % i, [128, 512], F32) for i in range(2)]
        def loads(i):
            t0 = i * 128
            mk.dma("sp", xr[i % 3][0][:], self.scr[src][t0:t0 + 128, :], [self.scrb[src]], [xr[i % 3][1]], xr[i % 3][1])
            mk.dma("sp", pin[i % 3][0][:], p_ap[t0:t0 + 128, :], [], [pin[i % 3][1]], pin[i % 3][1])
        loads(0)
        for i, t0 in enumerate(range(0, ntok, 128)):
            x, xb = xr[i % 3]
            xTt, xTb = xT[i % 3]
            p_, pb_ = pin[i % 3]
            pTt, pTb = pT[i % 3]
            if t0 + 128 < ntok:
                loads(i + 1)
            if i == 0:
                self.layer_norm(x, xb, stt, stb, w_bc, w_bcb, b_bc, b_bcb)
                self.transpose_to(x, xb, lambda j0, n: xTt[:, j0:j0 + n, :], xTb, KC, False)
                self.transpose_to(p_, pb_, lambda j0, n: pTt[:, j0:j0 + n, :], pTb, 2, False)
            if t0 + 128 < ntok:
                xn, xnb = xr[(i + 1) % 3]
                self.layer_norm(xn, xnb, stt, stb, w_bc, w_bcb, b_bc, b_bcb)
            for cb in range(4):
                cs = slice(cb * 512, (cb + 1) * 512)
                psg, psgb = self.bank()
                psp, pspb = self.bank()
                for kc in range(KC):
                    mk.op("pe", lambda e: e.matmul(psg[:, :], xTt[:, kc, :], wres[:, kc, cs], start=(kc == 0), stop=(kc == KC - 1)), [xTb, wresb], [psgb], sig=(kc == KC - 1))
                for kc in range(2):
                    mk.op("pe", lambda e: e.matmul(psp[:, :], pTt[:, kc, :], wpp[:, kc, cs], start=(kc == 0), stop=(kc == 1)), [pTb, wppb], [pspb], sig=(kc == 1))
                s_, s_b = sg[cb % 2]
                mk.op("act", lambda e: e.activation(out=s_[:], in_=psg[:, :], func=AF.Sigmoid), [psgb], [s_b])
                mk.op("dve", lambda e: e.tensor_tensor(out=s_[:], in0=s_[:], in1=psp[:, :], op=ALU.mult), [s_b, pspb], [s_b])
                mk.op("pool", lambda e: e.tensor_tensor(out=x[:, cs], in0=x[:, cs], in1=s_[:], op=ALU.add), [xb, s_b], [xb])
            mk.dma("sp", dst_ap[t0:t0 + 128, :], x[:], [xb], [dstb], xb)
            if t0 + 128 < ntok:
                xn, xnb = xr[(i + 1) % 3]
                xTn, xTnb = xT[(i + 1) % 3]
                pn, pnb = pin[(i + 1) % 3]
                pTn, pTnb = pT[(i + 1) % 3]
                self.transpose_to(xn, xnb, lambda j0, n: xTn[:, j0:j0 + n, :], xTnb, KC, False)
                self.transpose_to(pn, pnb, lambda j0, n: pTn[:, j0:j0 + n, :], pTnb, 2, False)
        mk.barrier()


@_p(Prog)
def layer0(self):
    self.l0_proj()
    self.l0_gates()
    self.l0_mlstm()
    self.l0_hgrn()
    self.dscr("XA", [F, D], F32)
    self.mix_ln("m0", "Y", self.inp["rec_w_out"], self.inp["x"], None, 0, F, "XA")
    self.dscr("R1", [F, D], F32)
    self.ffn_dense("XA", F, "R1")
    self.dscr("X1", [F, D], F32)
    self.ln_ple("p0", "R1", 0, F, self.scr["X1"], self.scrb["X1"], self.inp["p0"])


def declare_inputs(P, layer1=True):
    P.din("x", [T, D]); P.din("p0", [F, 256]); P.din("consts", [128, 512]); P.din("rmask", [128, T + CH])
    P.din("gbias", [8, 3]); P.din("lbl", [128, 32]); P.din("mlstm_norm_w", [4, 256]); P.din("hgrn_norm_w", [8, 128])
    P.din("ln_w", [4, D]); P.din("ln_b", [4, D]); P.din("w_in", [D, REC_IN]); P.din("rec_w_out", [D, D])
    P.din("ffn_w_gate", [D, FFN]); P.din("ffn_w_up", [D, FFN]); P.din("ffn_w_down", [FFN, D])
    P.din("ple_w_gate", [2 * D, D]); P.din("ple_w_proj", [512, D])
    if layer1:
        P.din("p1", [O, 256]); P.din("na_w_qkv", [D, 3 * D]); P.din("na_w_out", [D, D]); P.din("na_bias", [16 * 3 * 5 * 128, 128])
        P.din("moe_w_router", [D, NE]); P.din("moe_b_router", [1, NE])
        P.din("moe_w_gate", [NE * D, MOE]); P.din("moe_w_up", [NE * D, MOE]); P.din("moe_w_down", [NE * MOE, D])
        P.din("iota", [128, 1024])


def core_inputs(inp, core, layer1=True, shared=None):
    b, half = core // 2, core % 2
    flip = half == 1
    sh = shared if shared is not None else {}

    def cached(key, fn):
        if key not in sh:
            sh[key] = fn()
        return sh[key]
    f32 = lambda a: np.ascontiguousarray(a, dtype=np.float32)
    x = inp["x"][b]
    p = inp["p"][:, b]
    if flip:
        x = x[::-1]
        p = p[:, ::-1]
    o = {
        "x": f32(x), "p0": f32(p[0, :F]),
        "consts": cached("consts", make_consts), "rmask": cached("rmask", make_rmask),
        "gbias": make_gbias(inp["mlstm_gate_bias"][0], flip), "lbl": make_lbl(inp["hgrn_lb_logits"], flip),
        "mlstm_norm_w": f32(inp["mlstm_norm_w"][0].reshape(4, 256)), "hgrn_norm_w": f32(inp["hgrn_norm_w"][0].reshape(8, 128)),
        "ln_w": f32(inp["ln_w"].reshape(4, D)), "ln_b": f32(inp["ln_b"].reshape(4, D)),
        "w_in": cached(("w_in", flip), lambda: f32(inp["rec_w_in"][0][:, win_perm(flip)])),
        "rec_w_out": f32(inp["rec_w_out"][0]),
        "ffn_w_gate": f32(inp["ffn_w_gate"][0]), "ffn_w_up": f32(inp["ffn_w_up"][0]), "ffn_w_down": f32(inp["ffn_w_down"][0]),
        "ple_w_gate": f32(inp["ple_w_gate"].reshape(2 * D, D)), "ple_w_proj": f32(inp["ple_w_proj"].reshape(512, D)),
    }
    if layer1:
        o.update({
            "p1": f32(p[1, :O]), "na_w_qkv": f32(inp["na_w_qkv"][0]), "na_w_out": f32(inp["na_w_out"][0]),
            "na_bias": cached(("na_bias", flip), lambda: make_na_bias(inp["na_rpb"][0], flip)),
            "moe_w_router": f32(inp["moe_w_router"][0]), "moe_b_router": f32(inp["moe_b_router"][0].reshape(1, NE)),
            "moe_w_gate": f32(inp["moe_w_gate"][0].reshape(NE * D, MOE)), "moe_w_up": f32(inp["moe_w_up"][0].reshape(NE * D, MOE)),
            "moe_w_down": f32(inp["moe_w_down"][0].reshape(NE * MOE, D)),
            "iota": cached("iota", lambda: np.tile(np.arange(1024, dtype=np.float32)[None, :], (128, 1))),
        })
    return o


def make_na_bias(rpb, flip):
    out = np.full((16, 3, 5, 128, 128), -30000.0, np.float32)
    G = (lambda a: 63 - a) if flip else (lambda a: a)
    for k, p in ((0, 0), (1, 1), (2, 2)):
        ws = 0 if p < 2 else 2 * p - 4
        for c in range(5):
            ki = np.arange(128)
            kr = G(ws + 2 * c + ki // 64)[:, None]
            kc = G(ki % 64)[:, None]
            r = G(2 * p + ki // 64)[None, :]
            qc = G(ki % 64)[None, :]
            rs = np.clip(r - 4, 0, 56)
            vr = (kr >= rs) & (kr < rs + 8)
            cs = np.clip(qc - 8, 0, 48)
            vc = (kc >= cs) & (kc < cs + 16)
            dr = np.clip(kr - r + 7, 0, 14)
            dc = np.clip(kc - qc + 15, 0, 30)
            vals = rpb[:, dr, dc]
            out[:, k, c] = np.where((vr & vc)[None], vals, np.float32(-30000.0))
    return np.ascontiguousarray(out.reshape(16 * 3 * 5 * 128, 128))


@_p(Prog)
def l1_qkv(self):
    mk = self.mk
    S = self.dscr
    S("QT", [D, O], BF16); S("KT", [D, F], BF16); S("V1", [F, D], BF16)
    G = lambda c0, w, o, k, dst, dc0=0, scale=None: dict(c0=c0, w=w, orient=o, kind=k, dst=dst, dc0=dc0, scale=scale)
    with ExitStack() as st:
        xT, xTb = self.sb(st, "q_xT", [128, KC, F], BF16)
        xin = [self.sb(st, "q_xin%d" % i, [128, D], F32) for i in range(2)]
        wsl = [self.sb(st, "q_wp%d" % i, [128, KC, 512], BF16) for i in range(2)]
        stg = {False: [self.sb(st, "q_sg32_%d" % i, [128, 512], F32) for i in range(4)],
               True: [self.sb(st, "q_sg16_%d" % i, [128, 512], BF16) for i in range(4)]}
        self.build_xT(st, self.scr["X1"], self.scrb["X1"], xT, xTb, 0, F, xin)
        self.proj(st, xT, xTb, O, self.inp["na_w_qkv"], [G(0, D, "b", "scale", "QT", scale=128 ** -0.5)], 0, wsl, stg)
        self.proj(st, xT, xTb, F, self.inp["na_w_qkv"], [G(D, D, "b", "copy", "KT"), G(2 * D, D, "a", "copy", "V1")], 0, wsl, stg)
        mk.barrier()


@_p(Prog)
def l1_attn(self):
    mk = self.mk
    self.dscr("AO", [O, D], BF16)
    NP = O // 128
    with ExitStack() as st:
        KTs = [self.sb(st, "a_KT%d" % i, [128, F], BF16) for i in range(2)]
        QTs = [self.sb(st, "a_QT%d" % i, [128, O], BF16) for i in range(2)]
        Vs = [self.sb(st, "a_V%d" % i, [128, F // 128, 129], BF16) for i in range(2)]
        biass = [self.sb(st, "a_bias%d" % i, [128, 15, 128], F32) for i in range(2)]
        AOs = [self.sb(st, "a_AO%d" % i, [128, NP, 128], BF16) for i in range(2)]

        def loads(h):
            rows = slice(h * 128, (h + 1) * 128)
            KTh, KThb = KTs[h % 2]; QTh, QThb = QTs[h % 2]; V, Vb = Vs[h % 2]; bias, biasb = biass[h % 2]
            mk.dma("sp", KTh[:], self.scr["KT"][rows, :], [self.scrb["KT"]], [KThb], KThb)
            mk.dma("sp", QTh[:], self.scr["QT"][rows, :], [self.scrb["QT"]], [QThb], QThb)
            mk.dma("sp", V[:, :, 0:128], self.scr["V1"][:, rows].rearrange("(c p) e -> p c e", p=128), [self.scrb["V1"]], [Vb], Vb)
            mk.op("pool", lambda e: e.memset(V[:, :, 128:129], 1.0), [], [Vb])
            mk.dma("sp", bias[:], self.inp["na_bias"][h * 15 * 128:(h + 1) * 15 * 128, :].rearrange("(k p) q -> p k q", p=128), [], [biasb], biasb)
        loads(0)
        ssb = [self.sb(st, "a_s%d" % i, [128, 640], F32) for i in range(2)]
        pT = [self.sb(st, "a_pT%d" % i, [128, 640], BF16) for i in range(2)]
        rc = [self.sb(st, "a_rc%d" % i, [128, 1], F32) for i in range(4)]
        for h in range(16):
            rows = slice(h * 128, (h + 1) * 128)
            KTh, KThb = KTs[h % 2]; QTh, QThb = QTs[h % 2]; V, Vb = Vs[h % 2]; bias, biasb = biass[h % 2]
            AOh, AOhb = AOs[h % 2]
            if h + 1 < 16:
                loads(h + 1)
            def scores(p):
                ws = 0 if p < 2 else 2 * p - 4
                qs = slice(p * 128, (p + 1) * 128)
                for c in range(5):
                    kt0 = (ws // 2 + c) * 128
                    mk.op("pe", lambda e: e.matmul(self.psw[:, c * 128:(c + 1) * 128], KTh[:, kt0:kt0 + 128], QTh[:, qs], start=True, stop=True),
                          [KThb, QThb], [self.psb[4], self.psb[5]], sig=(c == 4))
            scores(0)
            for p in range(NP):
                k = p if p < 2 else 2
                ws = 0 if p < 2 else 2 * p - 4
                s_, s_b = ssb[p % 2]
                mk.op("dve", lambda e: e.tensor_tensor(out=s_[:].rearrange("p (c q) -> p c q", c=5), in0=self.psw[:, 0:640].rearrange("p (c q) -> p c q", c=5), in1=bias[:, k * 5:(k + 1) * 5, :], op=ALU.add),
                      [self.psb[4], self.psb[5], biasb], [s_b])
                pt, ptb = pT[p % 2]
                mk.op("act", lambda e: e.activation(out=pt[:], in_=s_[:], func=AF.Exp), [s_b], [ptb])
                if p + 1 < NP:
                    scores(p + 1)
                po, pob = self.bank()
                for c in range(5):
                    mk.op("pe", lambda e: e.matmul(po[:, 0:129], pt[:, c * 128:(c + 1) * 128], V[:, ws // 2 + c, :], start=(c == 0), stop=(c == 4)), [ptb, Vb], [pob], sig=(c == 4))
                r_, r_b = rc[p % 4]
                mk.op("dve", lambda e: e.reciprocal(out=r_[:], in_=po[:, 128:129]), [pob], [r_b])
                mk.op("act", lambda e: e.activation(out=AOh[:, p, :], in_=po[:, 0:128], func=AF.Copy, scale=r_[:, 0:1]), [pob, r_b], [AOhb])
            mk.dma("sp", self.scr["AO"][:, rows].rearrange("(c p) e -> p c e", p=128), AOh[:], [AOhb], [self.scrb["AO"]], AOhb)
        mk.barrier()


def _idma(self, out, in_, idx_ap, reads, writes, owner):
    need = self._deps(reads, writes)
    fresh = self._acquire(owner)
    if owner.dcnt and not fresh:
        k = ("d", owner)
        if need.get(k, 0) < owner.dcnt * 16:
            need[k] = owner.dcnt * 16
    self._wait("pool", need)
    ins = self.nc.gpsimd.indirect_dma_start(out=out, out_offset=None, in_=in_, in_offset=bass.IndirectOffsetOnAxis(ap=idx_ap, axis=0))
    owner.dcnt += 1
    ins.then_inc(owner.dsem, 16)
    self._record(("d", owner), owner.dcnt * 16, reads, writes)


MK.idma = _idma
NT_O = O // 128


@_p(Prog)
def l1_route(self):
    mk = self.mk
    S = self.dscr
    S("XG", [NE * D, CAP], BF16); S("IDXd", [128, 2 * NT_O], U32); S("Gd", [128, 2 * NT_O], F32)
    with ExitStack() as st:
        xcb, xcbb = self.sb(st, "r_xcb", [128, NT_O, D], BF16)
        wr, wrb = self.sb(st, "r_wr", [128, KC, NE], F32)
        br, brb = self.sb(st, "r_br", [128, NE], F32)
        ecap, ecapb = self.sb(st, "r_ecap", [128, NE], F32)
        iot, iotb = self.sb(st, "r_iot", [128, CAP], F32)
        tris, trisb = self.sb(st, "r_tris", [128, 128], BF16)
        ones, onesb = self.sb(st, "r_ones", [128, 128], BF16)
        ctmp, ctmpb = self.sb(st, "r_ctmp", [128, 256], F32)
        Mf, Mfb = self.sb(st, "r_Mf", [128, NT_O, NE], F32)
        Mbf, Mbfb = self.sb(st, "r_Mbf", [128, NT_O, NE], BF16)
        m1h, m1hb = self.sb(st, "r_m1h", [128, NT_O, NE], F32)
        m2h, m2hb = self.sb(st, "r_m2h", [128, NT_O, NE], F32)
        posf, posfb = self.sb(st, "r_pos", [128, NT_O, NE], F32)
        Gt, Gtb = self.sb(st, "r_G", [128, 2 * NT_O], F32)
        idxf, idxfb = self.sb(st, "r_idxf", [128, 2 * NT_O], F32)
        idxu, idxub = self.sb(st, "r_idxu", [128, 2 * NT_O], U32)
        xin = [self.sb(st, "r_x%d" % i, [128, D], F32) for i in range(2)]
        xT32 = [self.sb(st, "r_xT%d" % i, [128, KC, 128], F32) for i in range(2)]
        lg = [self.sb(st, "r_lg%d" % i, [128, 24], F32) for i in range(2)]
        Sels = [self.sb(st, "r_Sel%d" % i, [128, NT_O, CAP], BF16) for i in range(2)]
        xgs = [self.sb(st, "r_xg%d" % i, [128, CAP], BF16) for i in range(2)]
        mk.dma("sp", wr[:], self.inp["moe_w_router"].rearrange("(kc p) e -> p kc e", p=128), [], [wrb], wrb)
        mk.dma("sp", br[:], self.inp["moe_b_router"].to_broadcast([128, NE]), [], [brb], brb)
        mk.dma("sp", iot[:], self.inp["iota"][:, 0:CAP], [], [iotb], iotb)
        mk.dma("sp", ctmp[:], self.inp["consts"][:, 256:512], [], [ctmpb], ctmpb)
        mk.op("dve", lambda e: e.tensor_copy(out=tris[:], in_=ctmp[:, 0:128]), [ctmpb], [trisb])
        mk.op("dve", lambda e: e.tensor_copy(out=ones[:], in_=ctmp[:, 128:256]), [ctmpb], [onesb])
        mk.op("dve", lambda e: e.tensor_scalar(out=ecap[:], in0=iot[:, 0:NE], scalar1=float(CAP), scalar2=None, op0=ALU.mult), [iotb], [ecapb])
        for i in range(NT_O):
            x, xb = xin[i % 2]
            xT, xTb = xT32[i % 2]
            l_, l_b = lg[i % 2]
            mk.dma("sp", x[:], self.scr["XC"][i * 128:(i + 1) * 128, :], [self.scrb["XC"]], [xb], xb)
            mk.op("act", lambda e: e.copy(out=xcb[:, i, :], in_=x[:]), [xb], [xcbb])
            self.transpose_to(x, xb, lambda j0, n: xT[:, j0:j0 + n, :], xTb, KC, False)
            ps, psb = self.bank()
            for kc in range(KC):
                mk.op("pe", lambda e: e.matmul(ps[:, 0:NE], xT[:, kc, :], wr[:, kc, :], start=(kc == 0), stop=(kc == KC - 1)), [xTb, wrb], [psb], sig=(kc == KC - 1))
            mk.op("dve", lambda e: e.tensor_tensor(out=l_[:, 0:8], in0=ps[:, 0:NE], in1=br[:], op=ALU.add), [psb, brb], [l_b])
            mk.op("dve", lambda e: e.max(out=l_[:, 8:16], in_=l_[:, 0:8]), [l_b], [l_b])
            mk.op("dve", lambda e: e.tensor_scalar(out=Mf[:, i, :], in0=l_[:, 0:8], scalar1=l_[:, 9:10], scalar2=None, op0=ALU.is_ge), [l_b], [Mfb])
            mk.op("dve", lambda e: e.tensor_scalar(out=m1h[:, i, :], in0=l_[:, 0:8], scalar1=l_[:, 8:9], scalar2=None, op0=ALU.is_ge), [l_b], [m1hb])
            mk.op("dve", lambda e: e.tensor_tensor(out=m2h[:, i, :], in0=Mf[:, i, :], in1=m1h[:, i, :], op=ALU.subtract), [Mfb, m1hb], [m2hb])
            mk.op("dve", lambda e: e.tensor_tensor(out=l_[:, 16:17], in0=l_[:, 8:9], in1=l_[:, 9:10], op=ALU.subtract), [l_b], [l_b])
            mk.op("act", lambda e: e.activation(out=Gt[:, 2 * i:2 * i + 1], in_=l_[:, 16:17], func=AF.Sigmoid), [l_b], [Gtb])
            mk.op("dve", lambda e: e.tensor_scalar(out=Gt[:, 2 * i + 1:2 * i + 2], in0=Gt[:, 2 * i:2 * i + 1], scalar1=-1.0, scalar2=1.0, op0=ALU.mult, op1=ALU.add), [Gtb], [Gtb])
        mk.op("dve", lambda e: e.tensor_copy(out=Mbf[:], in_=Mf[:]), [Mfb], [Mbfb])
        for i in range(NT_O):
            ps, psb = self.bank()
            mk.op("pe", lambda e: e.matmul(ps[:, 0:NE], tris[:], Mbf[:, i, :], start=True, stop=(i == 0)), [trisb, Mbfb], [psb], sig=(i == 0))
            for i2 in range(i):
                mk.op("pe", lambda e: e.matmul(ps[:, 0:NE], ones[:], Mbf[:, i2, :], start=False, stop=(i2 == i - 1)), [onesb, Mbfb], [psb], sig=(i2 == i - 1))
            mk.op("act", lambda e: e.copy(out=posf[:, i, :], in_=ps[:, 0:NE]), [psb], [posfb])
            l_, l_b = lg[i % 2]
            for k, mh, mhb in ((0, m1h, m1hb), (1, m2h, m2hb)):
                mk.op("dve", lambda e: e.tensor_tensor(out=l_[:, 0:8], in0=posf[:, i, :], in1=ecap[:], op=ALU.add), [posfb, ecapb], [l_b])
                mk.op("dve", lambda e: e.tensor_tensor(out=l_[:, 0:8], in0=l_[:, 0:8], in1=mh[:, i, :], op=ALU.mult), [l_b, mhb], [l_b])
                mk.op("dve", lambda e: e.tensor_reduce(out=idxf[:, 2 * i + k:2 * i + k + 1], in_=l_[:, 0:8], axis=AX.X, op=ALU.add), [l_b], [idxfb])
        mk.op("dve", lambda e: e.tensor_copy(out=idxu[:], in_=idxf[:]), [idxfb], [idxub])
        mk.dma("sp", self.scr["IDXd"], idxu[:], [idxub], [self.scrb["IDXd"]], idxub)
        mk.dma("sp", self.scr["Gd"], Gt[:], [Gtb], [self.scrb["Gd"]], Gtb)
        for ex in range(NE):
            Sel, Selb = Sels[ex % 2]
            for i in range(NT_O):
                eng = "dve"
                mk.op(eng, lambda e: e.tensor_scalar(out=Sel[:, i, :], in0=iot[:], scalar1=posf[:, i, ex:ex + 1], scalar2=Mf[:, i, ex:ex + 1], op0=ALU.is_equal, op1=ALU.mult),
                      [iotb, posfb, Mfb], [Selb])
            for kc in range(KC):
                for i in range(NT_O):
                    mk.op("pe", lambda e: e.matmul(self.psw[:, 0:512], xcb[:, i, kc * 128:(kc + 1) * 128], Sel[:, i, 0:512], start=(i == 0), stop=(i == NT_O - 1)), [xcbb, Selb], [self.psb[4]], sig=False)
                    mk.op("pe", lambda e: e.matmul(self.psw[:, 512:CAP], xcb[:, i, kc * 128:(kc + 1) * 128], Sel[:, i, 512:CAP], start=(i == 0), stop=(i == NT_O - 1)), [xcbb, Selb], [self.psb[5]], sig=(i == NT_O - 1))
                xg, xgb = xgs[kc % 2]
                if kc % 2:
                    mk.op("act", lambda e: e.copy(out=xg[:], in_=self.psw[:, 0:CAP]), [self.psb[4], self.psb[5]], [xgb])
                else:
                    mk.op("dve", lambda e: e.tensor_copy(out=xg[:], in_=self.psw[:, 0:CAP]), [self.psb[4], self.psb[5]], [xgb])
                r0 = ex * D + kc * 128
                mk.dma("sp", self.scr["XG"][r0:r0 + 128, :], xg[:], [xgb], [self.scrb["XG"]], xgb)
        mk.barrier()


@_p(Prog)
def l1_experts(self):
    mk = self.mk
    self.dscr("YALL", [NE * CAP, D], F32)
    NH = MOE // 128
    NS = CAP // 128
    HB = CAP // 2
    Wg, Wu, Wd = self.inp["moe_w_gate"], self.inp["moe_w_up"], self.inp["moe_w_down"]
    with ExitStack() as st:
        xgT, xgTb = self.sb(st, "e_xgT", [128, KC, CAP], BF16)
        hT, hTb = self.sb(st, "e_hT", [128, NH, CAP], BF16)
        acc = [self.sb(st, "e_acc%d" % i, [128, D], F32) for i in range(NS)]
        wg = [self.sb(st, "e_wg%d" % i, [128, KC, 128], BF16) for i in range(2)]
        wu = [self.sb(st, "e_wu%d" % i, [128, KC, 128], BF16) for i in range(2)]
        wd = [self.sb(st, "e_wd%d" % i, [128, 8, 512], BF16) for i in range(2)]
        sg = [self.sb(st, "e_sg%d" % i, [128, HB], F32) for i in range(2)]
        for ex in range(NE):
            mk.dma("sp", xgT[:], self.scr["XG"][ex * D:(ex + 1) * D, :].rearrange("(kc p) s -> p kc s", p=128), [self.scrb["XG"]], [xgTb], xgTb)
            for j in range(NH):
                g_, gb_ = wg[j % 2]
                u_, ub_ = wu[j % 2]
                self.load_w_panel(g_, gb_, Wg[ex * D:(ex + 1) * D, :], j * 128, 128)
                self.load_w_panel(u_, ub_, Wu[ex * D:(ex + 1) * D, :], j * 128, 128)
                for blk in range(2):
                    bs = slice(blk * HB, (blk + 1) * HB)
                    psg, psgb = self.bank()
                    psu, psub = self.bank()
                    for kc in range(KC):
                        mk.op("pe", lambda e: e.matmul(psg[:, 0:HB], g_[:, kc, :], xgT[:, kc, bs], start=(kc == 0), stop=(kc == KC - 1)), [gb_, xgTb], [psgb], sig=(kc == KC - 1))
                    for kc in range(KC):
                        mk.op("pe", lambda e: e.matmul(psu[:, 0:HB], u_[:, kc, :], xgT[:, kc, bs], start=(kc == 0), stop=(kc == KC - 1)), [ub_, xgTb], [psub], sig=(kc == KC - 1))
                    s_, s_b = sg[blk]
                    mk.op("act", lambda e: e.activation(out=s_[:], in_=psg[:, 0:HB], func=AF.Silu), [psgb], [s_b])
                    mk.op("dve", lambda e: e.tensor_tensor(out=hT[:, j, bs], in0=s_[:], in1=psu[:, 0:HB], op=ALU.mult), [s_b, psub], [hTb])
            pi = 0
            for cb in range(4):
                cs = slice(cb * 512, (cb + 1) * 512)
                for j0 in range(0, NH, 8):
                    d_, db_ = wd[pi % 2]
                    pi += 1
                    r0 = ex * MOE + j0 * 128
                    mk.dma("pool", d_[:], Wd[r0:r0 + 1024, cs].rearrange("(j p) n -> p j n", p=128), [], [db_], db_)
                    for tt in range(NS):
                        a, ab = acc[tt]
                        ps, psb = self.bank()
                        for jj in range(8):
                            mk.op("pe", lambda e: e.matmul(ps[:, :], hT[:, j0 + jj, tt * 128:(tt + 1) * 128], d_[:, jj, :], start=(jj == 0), stop=(jj == 7)), [hTb, db_], [psb], sig=(jj == 7))
                        if j0 == 0:
                            mk.op("act", lambda e: e.copy(out=a[:, cs], in_=ps[:, :]), [psb], [ab])
                        else:
                            mk.op("dve", lambda e: e.tensor_tensor(out=a[:, cs], in0=a[:, cs], in1=ps[:, :], op=ALU.add), [ab, psb], [ab])
            for tt in range(NS):
                a, ab = acc[tt]
                r0 = ex * CAP + tt * 128
                mk.dma("sp", self.scr["YALL"][r0:r0 + 128, :], a[:], [ab], [self.scrb["YALL"]], ab)
        mk.barrier()


@_p(Prog)
def l1_combine(self):
    mk = self.mk
    self.dscr("R2", [O, D], F32)
    with ExitStack() as st:
        idx, idxb = self.sb(st, "c_idx", [128, 2 * NT_O], U32)
        g, gb = self.sb(st, "c_g", [128, 2 * NT_O], F32)
        xr = [self.sb(st, "c_x%d" % i, [128, D], F32) for i in range(3)]
        y1 = [self.sb(st, "c_y1%d" % i, [128, D], F32) for i in range(3)]
        y2 = [self.sb(st, "c_y2%d" % i, [128, D], F32) for i in range(3)]
        mk.dma("sp", idx[:], self.scr["IDXd"], [self.scrb["IDXd"]], [idxb], idxb)
        mk.dma("sp", g[:], self.scr["Gd"], [self.scrb["Gd"]], [gb], gb)
        def loads(i):
            mk.dma("sp", xr[i % 3][0][:], self.scr["XC"][i * 128:(i + 1) * 128, :], [self.scrb["XC"]], [xr[i % 3][1]], xr[i % 3][1])
            mk.idma(y1[i % 3][0][:], self.scr["YALL"], idx[:, 2 * i:2 * i + 1], [self.scrb["YALL"], idxb], [y1[i % 3][1]], y1[i % 3][1])
            mk.idma(y2[i % 3][0][:], self.scr["YALL"], idx[:, 2 * i + 1:2 * i + 2], [self.scrb["YALL"], idxb], [y2[i % 3][1]], y2[i % 3][1])
        loads(0)
        for i in range(NT_O):
            x, xb = xr[i % 3]
            a, ab = y1[i % 3]
            b, bb = y2[i % 3]
            if i + 1 < NT_O:
                loads(i + 1)
            mk.op("act", lambda e: e.mul(x[:], x[:], ALPHA), [xb], [xb])
            mk.op("dve", lambda e: e.scalar_tensor_tensor(out=x[:], in0=a[:], scalar=g[:, 2 * i:2 * i + 1], in1=x[:], op0=ALU.mult, op1=ALU.add), [ab, gb, xb], [xb])
            mk.op("dve", lambda e: e.scalar_tensor_tensor(out=x[:], in0=b[:], scalar=g[:, 2 * i + 1:2 * i + 2], in1=x[:], op0=ALU.mult, op1=ALU.add), [bb, gb, xb], [xb])
            mk.dma("sp", self.scr["R2"][i * 128:(i + 1) * 128, :], x[:], [xb], [self.scrb["R2"]], xb)
        mk.barrier()


@_p(Prog)
def layer1(self, out_ap, outb):
    self.l1_qkv()
    with ExitStack() as st:
        pre = self.preload_w(st, "m1", self.inp["na_w_out"])
        self.l1_attn()
        self.dscr("XC", [O, D], F32)
        self.mix_ln("m1", "AO", self.inp["na_w_out"], self.scr["X1"], self.scrb["X1"], 1, O, "XC", pre=pre)
    self.l1_route()
    self.l1_experts()
    with ExitStack() as st:
        pre = self.preload_w(st, "p1", self.inp["ple_w_gate"][D:2 * D, :])
        self.l1_combine()
        self.ln_ple("p1", "R2", 1, O, out_ap, outb, self.inp["p1"], pre=pre)


def build_full():
    nc = bass.Bass("TRN2", target_bir_lowering=False)
    with ExitStack() as es:
        P = Prog(nc, es)
        declare_inputs(P, layer1=True)
        out = nc.dram_tensor("out", [O, D], F32, kind="ExternalOutput").ap()
        outb = P.mk.buf("out")
        P.outs.append(outb)
        with ExitStack() as st:
            P.consts(st)
            P.small_consts(st)
            P.layer0()
            P.layer1(out, outb)
            P.mk.finish(P.outs)
    return nc


def kernel(**inputs):
    inp = {k: np.asarray(v) for k, v in inputs.items()}
    nc = build_full()
    shared = {}
    in_maps = [core_inputs(inp, c, layer1=True, shared=shared) for c in range(8)]
    for k in list(in_maps[0].keys()):
        for c in range(1, 8):
            a, b = in_maps[0][k], in_maps[c][k]
            if a is not b and a.shape == b.shape and k not in ("x", "p0", "p1", "gbias", "lbl", "w_in", "na_bias"):
                in_maps[c][k] = a
    res = run_bass_kernel_spmd(nc, in_maps, core_ids=list(range(8)))
    out = np.zeros((4, T, D), np.float32)
    for c in range(8):
        b, half = c // 2, c % 2
        oc = np.asarray(res.results[c]["out"], np.float32)
        if half == 0:
            out[b, 0:O] = oc
        else:
            out[b, O:T] = oc[::-1]
    return out
```
